# Optimizing a Trainium2 kernel written in Bass

```python
import jax, jax.numpy as jnp
from jax import lax
import numpy as np

D_MODEL = 2048
BATCH = 8
SEQ = 2048
DEPTH = 1

D_CONV = D_MODEL // 2
CONV_GROUPS = 16
CONV_WIDTH = 3
D_ATTN = D_MODEL - D_CONV
N_HEADS = 8
HEAD_DIM = D_ATTN // N_HEADS
D_MIX = D_CONV + D_ATTN
D_IN = 4 * D_CONV + 4 * D_ATTN
MOBA_BLOCK = 256
MOBA_TOPK = 3
Q_CHUNK = 64
EPS = 1e-6

kernel_name = "hymba_conv_moba_hybrid"


def rms_norm(x, gain):
    xf = x.astype(jnp.float32)
    y = xf * lax.rsqrt(jnp.mean(xf * xf, axis=-1, keepdims=True) + EPS)
    return (y * gain.astype(jnp.float32)).astype(x.dtype)


def short_gated_conv(h, b_gate, c_gate, conv_w):
    seq = h.shape[1]
    u = c_gate * h
    u_pad = jnp.pad(u, ((0, 0), (CONV_WIDTH - 1, 0), (0, 0)))
    conv = u_pad[:, 0:seq] * conv_w[0]
    for tap in range(1, CONV_WIDTH):
        conv = conv + u_pad[:, tap:tap + seq] * conv_w[tap]
    return b_gate * conv


def moba_attention(q, k, v):
    bsz, n_heads, seq, hd = q.shape
    n_blocks = -(-seq // MOBA_BLOCK)
    pad = n_blocks * MOBA_BLOCK - seq
    k_pad = jnp.pad(k, ((0, 0), (0, 0), (0, pad), (0, 0)))
    v_pad = jnp.pad(v, ((0, 0), (0, 0), (0, pad), (0, 0)))
    k_blocks = k_pad.reshape(bsz, n_heads, n_blocks, MOBA_BLOCK, hd)
    v_blocks = v_pad.reshape(bsz, n_heads, n_blocks, MOBA_BLOCK, hd)
    k_mean = jnp.mean(k_blocks.astype(jnp.float32), axis=3)
    k_sel = min(MOBA_TOPK, n_blocks)
    scale = hd ** -0.5
    n_chunks = seq // Q_CHUNK
    q_chunks = q.reshape(bsz, n_heads, n_chunks, Q_CHUNK, hd).transpose(2, 0, 1, 3, 4)
    b_idx = jnp.arange(bsz)[:, None, None]
    h_idx = jnp.arange(n_heads)[None, :, None]
    block_ids = jnp.arange(n_blocks)
    slot_ids = jnp.arange(k_sel)
    key_offsets = jnp.arange(MOBA_BLOCK)

    def chunk_fn(args):
        ci, q_blk = args
        start = ci * Q_CHUNK
        own = start // MOBA_BLOCK
        q_pos = start + jnp.arange(Q_CHUNK)
        gate = jnp.einsum('bhqd,bhnd->bhqn', q_blk.astype(jnp.float32), k_mean)
        gate = jnp.where(block_ids < own, gate, -jnp.inf)
        _, sel = lax.top_k(gate, k_sel)
        sel_valid = slot_ids < own
        k_own = lax.dynamic_index_in_dim(k_blocks, own, axis=2, keepdims=False)
        v_own = lax.dynamic_index_in_dim(v_blocks, own, axis=2, keepdims=False)
        s_own = jnp.einsum('bhqd,bhkd->bhqk', q_blk, k_own).astype(jnp.float32) * scale
        k_pos = own * MOBA_BLOCK + key_offsets
        s_own = jnp.where(k_pos[None, :] <= q_pos[:, None], s_own, -jnp.inf)
        scores = []
        for slot in range(k_sel):
            k_g = k_blocks[b_idx, h_idx, sel[..., slot]]
            s = jnp.einsum('bhqd,bhqkd->bhqk', q_blk, k_g).astype(jnp.float32) * scale
            scores.append(jnp.where(sel_valid[slot], s, -jnp.inf))
        scores.append(s_own)
        probs = jax.nn.softmax(jnp.concatenate(scores, axis=-1), axis=-1).astype(v.dtype)
        out = jnp.einsum('bhqk,bhkd->bhqd', probs[..., k_sel * MOBA_BLOCK:], v_own)
        for slot in range(k_sel):
            v_g = v_blocks[b_idx, h_idx, sel[..., slot]]
            p_slot = probs[..., slot * MOBA_BLOCK:(slot + 1) * MOBA_BLOCK]
            out = out + jnp.einsum('bhqk,bhqkd->bhqd', p_slot, v_g)
        return out.astype(q.dtype)

    outs = lax.map(chunk_fn, (jnp.arange(n_chunks), q_chunks))
    return outs.transpose(1, 2, 0, 3, 4).reshape(bsz, n_heads, seq, hd)


def setup_inputs(seed: int = 0) -> dict:
    key = jax.random.key(seed)
    ks = jax.random.split(key, 9)
    x = jax.random.normal(ks[0], (BATCH, SEQ, D_MODEL), jnp.float32)
    norm_gain = 1.0 + 0.1 * jax.random.normal(ks[1], (DEPTH, D_MODEL), jnp.float32)
    w_in = jax.random.normal(ks[2], (DEPTH, D_MODEL, D_IN), jnp.float32) * D_MODEL ** -0.5
    conv_w = jax.random.normal(ks[3], (DEPTH, CONV_WIDTH, D_CONV), jnp.float32) * CONV_WIDTH ** -0.5
    q_norm_gain = 1.0 + 0.1 * jax.random.normal(ks[4], (DEPTH, HEAD_DIM), jnp.float32)
    k_norm_gain = 1.0 + 0.1 * jax.random.normal(ks[5], (DEPTH, HEAD_DIM), jnp.float32)
    conv_out_gain = 1.0 + 0.1 * jax.random.normal(ks[6], (DEPTH, D_CONV), jnp.float32)
    attn_out_gain = 1.0 + 0.1 * jax.random.normal(ks[7], (DEPTH, D_ATTN), jnp.float32)
    w_out = jax.random.normal(ks[8], (DEPTH, D_MIX, D_MODEL), jnp.float32) * D_MIX ** -0.5
    return {"x": x, "norm_gain": norm_gain, "w_in": w_in, "conv_w": conv_w,
            "q_norm_gain": q_norm_gain, "k_norm_gain": k_norm_gain,
            "conv_out_gain": conv_out_gain, "attn_out_gain": attn_out_gain, "w_out": w_out}


def reference(x, norm_gain, w_in, conv_w, q_norm_gain, k_norm_gain, conv_out_gain, attn_out_gain, w_out):
    bsz, seq, _ = x.shape
    split_points = [D_CONV, 2 * D_CONV, 3 * D_CONV, 4 * D_CONV,
                    4 * D_CONV + D_ATTN, 4 * D_CONV + 2 * D_ATTN, 4 * D_CONV + 3 * D_ATTN]
    for layer in range(DEPTH):
        h = rms_norm(x, norm_gain[layer])
        proj = jnp.einsum('bsd,de->bse', h, w_in[layer])
        h_conv, b_gate, c_gate, z_conv, q, k, v, z_attn = jnp.split(proj, split_points, axis=-1)
        y_conv = short_gated_conv(h_conv, b_gate, c_gate, conv_w[layer])
        y_conv = rms_norm(y_conv, conv_out_gain[layer]) * jax.nn.silu(z_conv)
        q = rms_norm(q.reshape(bsz, seq, N_HEADS, HEAD_DIM), q_norm_gain[layer]).transpose(0, 2, 1, 3)
        k = rms_norm(k.reshape(bsz, seq, N_HEADS, HEAD_DIM), k_norm_gain[layer]).transpose(0, 2, 1, 3)
        v = v.reshape(bsz, seq, N_HEADS, HEAD_DIM).transpose(0, 2, 1, 3)
        y_attn = moba_attention(q, k, v).transpose(0, 2, 1, 3).reshape(bsz, seq, D_ATTN)
        y_attn = rms_norm(y_attn, attn_out_gain[layer]) * jax.nn.silu(z_attn)
        y = jnp.concatenate([y_conv, y_attn], axis=-1)
        x = x + jnp.einsum('bse,ed->bsd', y, w_out[layer])
    return x
```

```python
import numpy as np
import concourse.bass as bass
import concourse.mybir as mybir
from concourse.bass_utils import run_bass_kernel_spmd

F32 = mybir.dt.float32
BF16 = mybir.dt.bfloat16
AF = mybir.ActivationFunctionType
ALU = mybir.AluOpType
AX = mybir.AxisListType

P = 128
S = 2048
BLK = 256
NBLK = S // BLK
TOPK = 3
BIG = 32768.0
EPS = 1e-6
NPC = S // 512
NT = S // P
NSLOT = 4
NDMA = 8

K_ID, K_TRI, K_SEL, K_CAND, K_FIX = 0, 128, 256, 256 + 1024, 256 + 1024 + 64
NKST = K_FIX + 64


class Task:
    __slots__ = ("eng", "fn", "deps", "pos", "sig", "sigval", "dma", "dsem", "dval", "dprev", "name")

    def __init__(self, eng, fn, dma, name):
        self.eng = eng
        self.fn = fn
        self.dma = dma
        self.name = name
        self.deps = set()
        self.pos = -1
        self.sig = False
        self.sigval = 0
        self.dsem = None
        self.dval = 0
        self.dprev = None


class Sched:
    ENGS = ("pe", "act", "dve", "pool", "sp")

    def __init__(self):
        self.tasks = {e: [] for e in self.ENGS}
        self.last_w = {}
        self.readers = {}
        self.ndma = {e: 0 for e in self.ENGS}

    def add(self, eng, fn, reads=(), writes=(), dma=False, name="", extra=()):
        t = Task(eng, fn, dma, name)
        deps = set(extra)
        for k in reads:
            w = self.last_w.get(k)
            if w is not None:
                deps.add(w)
        for k in writes:
            w = self.last_w.get(k)
            if w is not None:
                deps.add(w)
            for r in self.readers.get(k, ()):
                deps.add(r)
        for k in reads:
            self.readers.setdefault(k, []).append(t)
        for k in writes:
            self.last_w[k] = t
            self.readers[k] = []
        deps.discard(t)
        t.pos = len(self.tasks[eng])
        self.tasks[eng].append(t)
        best = {}
        keep = []
        for d in deps:
            if d.dma:
                keep.append(d)
                continue
            if d.eng == "pe" and eng == "pe" and not dma:
                continue
            b = best.get(d.eng)
            if b is None or d.pos > b.pos:
                best[d.eng] = d
        keep.extend(best.values())
        t.deps = keep
        for d in keep:
            d.sig = True
        if dma:
            t.sig = True
        return t

    def last(self, eng):
        ts = self.tasks[eng]
        return ts[-1] if ts else None

    def barrier(self, engs):
        lasts = [self.last(e) for e in self.ENGS]
        lasts = [t for t in lasts if t is not None]
        for e in engs:
            self.add(e, None, extra=[t for t in lasts if t.eng != e], name="barrier")

    def finalize(self, csem, dsems):
        for e in self.ENGS:
            cnt = 0
            nd = 0
            for t in self.tasks[e]:
                if t.dma:
                    t.dsem = dsems[e][nd % NDMA]
                    t.dval = 16 * (nd // NDMA + 1)
                    nd += 1
                elif t.sig and t.fn is not None:
                    cnt += 1
                    t.sigval = cnt
                elif t.sig:
                    raise RuntimeError("no-op task used as dependency")
        self.csem = csem

    def emit(self, e, h):
        waited = {}

        def wait(sem, key, val):
            if waited.get(key, 0) < val:
                h.wait_ge(sem, val)
                waited[key] = val

        nd = 0
        for t in self.tasks[e]:
            for d in t.deps:
                if d.dma:
                    wait(d.dsem, ("d", d.eng, id(d.dsem)), d.dval)
                else:
                    wait(self.csem[d.eng], ("c", d.eng), d.sigval)
            if t.dma:
                if t.dval > 16:
                    wait(t.dsem, ("d", e, id(t.dsem)), t.dval - 16)
                ins = t.fn(h)
                ins.then_inc(t.dsem, 16)
                nd += 1
            elif t.fn is not None:
                ins = t.fn(h)
                if t.sig:
                    ins.then_inc(self.csem[e], 1)


def build(D, dbg=()):
    KC = D // P
    CC = KC // 2
    H = KC // 2
    NCH = 4 * KC
    ND = D // 512
    NCST = KC + 3 * CC + CC + H + 2
    C_GX, C_CW, C_GC, C_GA, C_GQ, C_GK = 0, KC, KC + 3 * CC, KC + 4 * CC, KC + 4 * CC + H, KC + 4 * CC + H + 1

    nc = bass.Bass("TRN2", target_bir_lowering=False)
    xT_d = nc.dram_tensor("xT", [D, S], F32, kind="ExternalInput").ap()
    x_d = nc.dram_tensor("x", [S, D], F32, kind="ExternalInput").ap()
    win_d = nc.dram_tensor("w_in", [NCH, P, KC * P], F32, kind="ExternalInput").ap()
    wout_d = nc.dram_tensor("w_out", [ND, P, KC * 512], F32, kind="ExternalInput").ap()
    cst_d = nc.dram_tensor("cst", [P, NCST], F32, kind="ExternalInput").ap()
    kst_d = nc.dram_tensor("kst", [P, NKST], F32, kind="ExternalInput").ap()
    out_d = nc.dram_tensor("out", [S, D], F32, kind="ExternalOutput").ap()
    dbg_d = {}

    SB_BASE, SB_END = 16512, 229344
    off = [SB_BASE]

    def alloc(name, shape, dt, at=None):
        nbytes = int(np.prod(shape[1:])) * (4 if dt == F32 else 2)
        if at is None:
            at = off[0]
            off[0] = (at + nbytes + 31) // 32 * 32
        return nc.alloc_sbuf_tensor_at(name, list(shape), dt, offset=at)

    HT_OFF = SB_BASE
    hT = alloc("hT", [P, KC, S], BF16)
    YT_OFF = off[0]
    yT = alloc("yT", [P, KC, S], BF16)
    wring = alloc("wring", [P, NSLOT, KC * P], BF16)
    cst = alloc("cst_sb", [P, NCST], F32)
    ident_bf = alloc("ident_bf", [P, P], BF16)
    tri_bf = alloc("tri_bf", [P, P], BF16)
    ones_bf = alloc("ones_bf", [P, P], BF16)
    selj_bf = alloc("selj_bf", [P, NBLK * P], BF16)
    ident_f = alloc("ident_f", [P, P], F32)
    cand_f = alloc("cand_f", [P, 64], F32)
    fixc_f = alloc("fixc_f", [P, 64], F32)
    nbias = alloc("nbias", [P, 1], F32)
    gqs = alloc("gqs", [P, 2], F32)
    stat_sb = alloc("stat_sb", [P, 2 * NT], F32)
    kms = alloc("kms", [P, NBLK], F32)
    kmean_bf = alloc("kmean_bf", [P, NBLK], BF16)
    ones_f = alloc("ones_f", [P, P], F32)
    SCR = off[0]
    off[0] = SCR
    hs = alloc("hs", [P, NPC, 512], F32)
    u = alloc("u", [P, 2 + S + 6], F32)
    a_t = alloc("a_t", [P, NPC, 512], F32)
    y_t = alloc("y_t", [P, 2, 512], F32)
    sqc = alloc("sqc", [P, 2, 512], BF16)
    sz1 = alloc("sz1", [P, NPC, 512], F32)
    sq0 = alloc("sq0", [P, 2, 4, 512], BF16)
    rstd = alloc("rstd", [P, 2, 512], F32)
    prm = alloc("prm", [P, P], F32)
    onesf = alloc("onesf", [P, P], F32)
    END1 = off[0]
    off[0] = SCR
    qf = alloc("qf", [P, 2, 512], F32)
    sqq = alloc("sqq", [P, 2, 512], BF16)
    rq = alloc("rq", [P, 1, 512], F32)
    qT = alloc("qT", [P, 2, S], BF16)
    kT = alloc("kT", [P, 2, S], BF16)
    Vt = alloc("Vt", [P, 2, NT, P], BF16)
    zs = alloc("zs", [P, 2, S], BF16)
    pT = alloc("pT", [P, 3, 512], BF16)
    rden = alloc("rden", [P, 1, 512], F32)
    acc = alloc("acc", [P, 512], F32)
    at_t = alloc("at_t", [P, 1, 512], F32)
    sqa = alloc("sqa", [P, 2, 512], BF16)
    gm = alloc("gm", [P, 64], F32)
    top8 = alloc("top8", [P, 8, 8], F32)
    negm_pad = alloc("negm_pad", [P, 8, P], BF16)
    negmT = alloc("negmT", [P, 2, 8 * P], BF16)
    END2 = off[0]
    assert max(END1, END2) <= SB_END, (END1, END2)
    WO_BYTES = KC * 512 * 2
    wo = [alloc("wo%d" % i, [P, KC, 512], BF16, at=HT_OFF + i * WO_BYTES) for i in range(2)]
    p3 = HT_OFF + 2 * WO_BYTES
    XR = 4 if KC >= 16 else 2
    xres = alloc("xres", [P, XR, 512], F32, at=p3)
    t1 = alloc("t1", [P, 2, 512], F32, at=p3 + XR * 2048)
    ot = alloc("ot", [P, 3, 512], F32, at=p3 + XR * 2048 + 4096)
    assert p3 + XR * 2048 + 4096 + 6144 <= HT_OFF + KC * S * 2
    xs = alloc("xs", [P, 2, KC, 512], F32, at=YT_OFF)
    assert 2 * KC * 512 * 4 <= KC * S * 2

    ps = [nc.alloc_psum_tensor("ps%d" % i, [P, 512], F32) for i in range(8)]
    ps2_bf = ps[2].bitcast(BF16)
    PB_MISC, PB_STAT = 2, 7

    sc = Sched()

    def k_hT(k, c):
        return ("hT", k, c)

    def k_yT(e, c):
        return ("yT", e, c)

    def ld(eng, out_ap, in_ap, writes, reads=(), extra=()):
        return sc.add(eng, lambda h, o=out_ap, i=in_ap: h.dma_start(out=o, in_=i), reads=reads, writes=writes, dma=True, extra=extra)

    ld("sp", cst[:, :], cst_d[:, :], [("cst",)])
    ld("sp", ident_f[:, :], kst_d[:, K_ID:K_ID + P], [("ident_f",)])
    ld("sp", cand_f[:, :], kst_d[:, K_CAND:K_CAND + 64], [("cand",)])
    ld("sp", fixc_f[:, :], kst_d[:, K_FIX:K_FIX + 64], [("fixc",)])
    ld("pool", ident_bf[:, :], kst_d[:, K_ID:K_ID + P], [("ident_bf",)])
    ld("pool", tri_bf[:, :], kst_d[:, K_TRI:K_TRI + P], [("tri",)])
    ld("pool", selj_bf[:, :], kst_d[:, K_SEL:K_SEL + NBLK * P], [("selj",)])
    sc.add("dve", lambda h: h.memset(ones_bf[:, :], 1.0), writes=[("ones",)])
    sc.add("dve", lambda h: h.memset(onesf[:, :], 1.0), writes=[("onesf",)])
    sc.add("dve", lambda h: h.memset(ones_f[:, :], 1.0), writes=[("ones_f",)])
    sc.add("dve", lambda h: h.memset(u[:, 0:2], 0.0), writes=[("u", -1)])

    seq = []
    for cc in range(CC):
        seq += [cc, 2 * CC + cc, 3 * CC + cc, CC + cc]
    for hh in range(H):
        seq += [4 * CC + hh, 4 * CC + H + hh, 4 * CC + 2 * H + hh, 4 * CC + 3 * H + hh]
    wnext = [0]

    def issue_w(extra=()):
        i = wnext[0]
        if i >= len(seq):
            return
        wnext[0] += 1
        slot = i % NSLOT
        ld("pool", wring[:, slot, :], win_d[seq[i], :, :], [("w", slot)], extra=extra)

    issue_w()

    sc.add("dve", lambda h: h.tensor_tensor(out=gqs[:, 0:1], in0=cst[:, C_GQ:C_GQ + 1], in1=cst[:, C_GK:C_GK + 1], op=ALU.mult),
           reads=[("cst",)], writes=[("gqs",)])
    sc.add("dve", lambda h: h.tensor_scalar(out=prm[:, :], in0=ident_f[:, :], scalar1=gqs[:, 0:1], scalar2=None, op0=ALU.mult),
           reads=[("gqs",), ("ident_f",)], writes=[("prm",)])
    sc.add("pe", lambda h: h.matmul(ps[5][:, 0:P], lhsT=onesf[:, :], rhs=prm[:, :], start=True, stop=True),
           reads=[("prm",), ("onesf",)], writes=[("ps", 5)])
    sc.add("dve", lambda h: h.tensor_reduce(out=gqs[:, 1:2], in_=ps[5][:, 0:P], axis=AX.X, op=ALU.max, apply_absolute_value=True),
           reads=[("ps", 5)], writes=[("gqs2",)])
    sc.add("dve", lambda h: h.tensor_scalar(out=nbias[:, :], in0=gqs[:, 1:2], scalar1=-float(np.sqrt(P)), scalar2=None, op0=ALU.mult),
           reads=[("gqs2",)], writes=[("nbias",)])

    xT_r = xT_d.rearrange("(k p) t -> p k t", p=P)
    KG = KC // 4
    def prologue_chunk(c):
        half = c % 2
        xl = []
        for g in range(KG):
            xl.append(ld("sp", xs[:, half, 4 * g:4 * g + 4, :], xT_r[:, 4 * g:4 * g + 4, c * 512:(c + 1) * 512],
                         [("xs", half, g)]))
        if c == 0:
            for _ in range(NSLOT - 1):
                issue_w(extra=xl)
        for g in range(KG):
            b = (c * KG + g) % 2
            sc.add("act", lambda h, b=b, half=half, g=g: h.activation(out=sq0[:, b, :, :], in_=xs[:, half, 4 * g:4 * g + 4, :], func=AF.Square),
                   reads=[("xs", half, g)], writes=[("sq0", b)])

            def f(h, b=b, g=g):
                ins = None
                for kk in range(4):
                    ins = h.matmul(ps[6][:, :], lhsT=ones_bf[:, :], rhs=sq0[:, b, kk, :],
                                   start=(g == 0 and kk == 0), stop=(g == KG - 1 and kk == 3))
                return ins
            sc.add("pe", f, reads=[("sq0", b), ("ones",)], writes=[("ps", 6)])
        r = c % 2
        sc.add("act", lambda h, r=r: h.activation(out=rstd[:, r, :], in_=ps[6][:, :], func=AF.Ln, scale=1.0 / D, bias=EPS),
               reads=[("ps", 6)], writes=[("rstd", r)])
        sc.add("act", lambda h, r=r: h.activation(out=rstd[:, r, :], in_=rstd[:, r, :], func=AF.Exp, scale=-0.5),
               reads=[("rstd", r)], writes=[("rstd", r)])
        for k in range(KC):
            sc.add("dve", lambda h, k=k, c=c, half=half, r=r: h.scalar_tensor_tensor(
                out=hT[:, k, c * 512:(c + 1) * 512], in0=xs[:, half, k, :], scalar=cst[:, C_GX + k:C_GX + k + 1],
                in1=rstd[:, r, :], op0=ALU.mult, op1=ALU.mult),
                reads=[("xs", half, k // 4), ("rstd", r), ("cst",)], writes=[k_hT(k, c)])

    def xs_readers():
        d = set()
        for half_ in range(2):
            for g_ in range(KG):
                d |= set(sc.readers.get(("xs", half_, g_), ()))
                w_ = sc.last_w.get(("xs", half_, g_))
                if w_ is not None:
                    d.add(w_)
        return list(d)

    pend = []

    def defer(fn, reads, writes, age=1, then=None):
        pend.append([age, fn, reads, writes, then])

    def flush(all_=False):
        keep = []
        todo = list(pend)
        pend[:] = []
        for it in todo:
            if it[0] <= 0 or all_:
                sc.add("pe", it[1], reads=it[2], writes=it[3])
                if it[4] is not None:
                    it[4]()
            else:
                it[0] -= 1
                keep.append(it)
        pend[:] = keep + pend

    widx = [0]

    def slab_gen(banks, on_group, v_mode=False):
        i = widx[0]
        widx[0] += 1
        slot = i % NSLOT
        KH = KC // 2
        for c in range(NPC):
            bank = banks()
            for half in range(2):
                if not v_mode:
                    def f(h, c=c, bank=bank, slot=slot, half=half):
                        ins = None
                        for k in range(half * KH, (half + 1) * KH):
                            ins = h.matmul(ps[bank][:, :], lhsT=wring[:, slot, k * P:(k + 1) * P], rhs=hT[:, k, c * 512:(c + 1) * 512],
                                           start=(k == 0), stop=(k == KC - 1))
                        return ins
                else:
                    def f(h, c=c, bank=bank, slot=slot, half=half):
                        ins = None
                        for tt in range(2 * half, 2 * half + 2):
                            t = 4 * c + tt
                            for k in range(KC):
                                ins = h.matmul(ps[bank][:, tt * P:(tt + 1) * P], lhsT=hT[:, k, t * P:(t + 1) * P],
                                               rhs=wring[:, slot, k * P:(k + 1) * P], start=(k == 0), stop=(k == KC - 1))
                        return ins
                sc.add("pe", f, reads=[("w", slot)] + [k_hT(k, c) for k in range(KC)], writes=[("ps", bank)])
                if half == 0:
                    yield
            flush()
            on_group(c, bank)
            if c == NPC - 1:
                issue_w()
            yield

    def slab(banks, on_group, v_mode=False):
        for _ in slab_gen(banks, on_group, v_mode):
            pass

    def rr(banks_list):
        st = [0]

        def nxt():
            b = banks_list[st[0] % len(banks_list)]
            st[0] += 1
            return b
        return nxt

    stat_started = [False]

    def stats_mm(src, col0):
        def f(h, src=src, col0=col0):
            ins = None
            for tt in range(4):
                first = not stat_started[0]
                stat_started[0] = True
                ins = h.matmul(ps[PB_STAT][:, col0 + tt:col0 + tt + 1], lhsT=src[:, tt * P:(tt + 1) * P], rhs=ones_bf[:, 0:1],
                               start=first, stop=True, skip_group_check=True)
            return ins
        return f

    banks1 = rr([0, 1, 2, 3, 4, 5])
    for cc in range(CC):
        def on_h(c, bank):
            sc.add("act", lambda h, c=c, bank=bank: h.activation(out=hs[:, c, :], in_=ps[bank][:, :], func=AF.Copy),
                   reads=[("ps", bank)], writes=[("hs", c)])

        def on_c(c, bank, cc=cc):
            sc.add("dve", lambda h, c=c, bank=bank: h.tensor_tensor(out=u[:, 2 + c * 512:2 + (c + 1) * 512], in0=ps[bank][:, :],
                                                                    in1=hs[:, c, :], op=ALU.mult),
                   reads=[("ps", bank), ("hs", c)], writes=[("u", c)])
            r = c
            w0 = cst[:, C_CW + 3 * cc + 0:C_CW + 3 * cc + 1]
            w1 = cst[:, C_CW + 3 * cc + 1:C_CW + 3 * cc + 2]
            w2 = cst[:, C_CW + 3 * cc + 2:C_CW + 3 * cc + 3]
            sc.add("act", lambda h, c=c, r=r, w2=w2: h.activation(out=a_t[:, r, :], in_=u[:, 2 + c * 512:2 + (c + 1) * 512],
                                                                   func=AF.Identity, scale=w2),
                   reads=[("u", c), ("cst",)], writes=[("a", r)])
            sc.add("dve", lambda h, c=c, r=r, w1=w1: h.scalar_tensor_tensor(out=a_t[:, r, :], in0=u[:, 1 + c * 512:1 + (c + 1) * 512],
                                                                            scalar=w1, in1=a_t[:, r, :], op0=ALU.mult, op1=ALU.add),
                   reads=[("u", c), ("u", c - 1), ("a", r)], writes=[("a", r)])
            sc.add("dve", lambda h, c=c, r=r, w0=w0: h.scalar_tensor_tensor(out=a_t[:, r, :], in0=u[:, c * 512:(c + 1) * 512],
                                                                            scalar=w0, in1=a_t[:, r, :], op0=ALU.mult, op1=ALU.add),
                   reads=[("u", c), ("u", c - 1), ("a", r)], writes=[("a", r)])

        def on_z(c, bank):
            sc.add("act", lambda h, c=c, bank=bank: h.activation(out=sz1[:, c, :], in_=ps[bank][:, :], func=AF.Silu),
                   reads=[("ps", bank)], writes=[("sz1", c)])

        def on_b(c, bank, cc=cc):
            r = c % 2
            sc.add("dve", lambda h, r=r, c=c, bank=bank: h.tensor_tensor(out=y_t[:, r, :], in0=ps[bank][:, :], in1=a_t[:, c, :], op=ALU.mult),
                   reads=[("ps", bank), ("a", c)], writes=[("y", r)])
            sc.add("act", lambda h, r=r: h.activation(out=sqc[:, r, :], in_=y_t[:, r, :], func=AF.Square),
                   reads=[("y", r)], writes=[("sqc", r)])
            defer(stats_mm(sqc[:, r, :], 4 * c), [("sqc", r), ("ones",)], [("ps", PB_STAT)], age=1)
            gcc = cst[:, C_GC + cc:C_GC + cc + 1]
            sc.add("dve", lambda h, r=r, c=c, cc=cc, gcc=gcc: h.scalar_tensor_tensor(
                out=yT[:, cc, c * 512:(c + 1) * 512], in0=y_t[:, r, :], scalar=gcc, in1=sz1[:, c, :], op0=ALU.mult, op1=ALU.mult),
                reads=[("y", r), ("sz1", c), ("cst",)], writes=[k_yT(cc, c)], extra=xs_readers())

        if cc == 0:
            g_h = slab_gen(banks1, on_h)
            g_c = slab_gen(banks1, on_c)
            prologue_chunk(0)
            for c in range(NPC):
                if c + 1 < NPC:
                    prologue_chunk(c + 1)
                next(g_h, None)
                next(g_h, None)
                next(g_c, None)
                next(g_c, None)
            for _ in g_h:
                pass
            for _ in g_c:
                pass
        else:
            slab(banks1, on_h)
            slab(banks1, on_c)
        slab(banks1, on_z)
        slab(banks1, on_b)
    flush(all_=True)

    if "p1" in dbg:
        outs = []
        for name, src in (("hs", hs), ("u", u), ("a_t", a_t), ("y_t", y_t), ("sz1", sz1), ("hT", hT), ("yT", yT), ("cst", cst)):
            shp = list(src.shape)
            d = nc.dram_tensor("dbg_" + name, shp, src.dtype, kind="ExternalOutput").ap()
            full = tuple(slice(None) for _ in shp)
            sc.barrier(["sp"])
            outs.append(sc.add("sp", lambda h, d=d, src=src, full=full: h.dma_start(out=d[full], in_=src[full]), dma=True))
        sc.add("sp", None, extra=outs, name="final")
        return _emit(nc, sc)
    sc.barrier(["act", "dve"])
    sc.add("dve", lambda h: h.memset(negm_pad[:, :, :], 0.0), writes=[("negm_pad",)])

    banks2 = rr([0, 1])
    sbanks = rr([3, 4])
    PB_O, PB_D = 5, 6
    prr = [0]
    scale = float(P) ** -0.5

    def proj_steps(hh):
        hp = hh % 2
        for which, dst, gcol in ((0, qT, C_GQ), (1, kT, C_GK)):
            key = "qT" if which == 0 else "kT"

            def on(c, bank, dst=dst, gcol=gcol, key=key):
                r = c % 2
                sc.add("act", lambda h, r=r, bank=bank: h.activation(out=qf[:, r, :], in_=ps[bank][:, :], func=AF.Copy),
                       reads=[("ps", bank)], writes=[("qf", r)])
                sc.add("act", lambda h, r=r, bank=bank: h.activation(out=sqq[:, r, :], in_=ps[bank][:, :], func=AF.Square),
                       reads=[("ps", bank)], writes=[("sqq", r)])

                def post(r=r, c=c, dst=dst, gcol=gcol, key=key):
                    sc.add("act", lambda h, r=r: h.activation(out=rq[:, 0, :], in_=ps[PB_MISC][:, :], func=AF.Ln, scale=1.0 / P, bias=EPS),
                           reads=[("ps", PB_MISC)], writes=[("rq",)])
                    sc.add("act", lambda h, r=r: h.activation(out=rq[:, 0, :], in_=rq[:, 0, :], func=AF.Exp, scale=-0.5),
                           reads=[("rq",)], writes=[("rq",)])
                    sc.add("dve", lambda h, r=r, c=c, dst=dst, gcol=gcol: h.scalar_tensor_tensor(
                        out=dst[:, hp, c * 512:(c + 1) * 512], in0=qf[:, r, :], scalar=cst[:, gcol:gcol + 1], in1=rq[:, 0, :],
                        op0=ALU.mult, op1=ALU.mult),
                        reads=[("qf", r), ("rq",), ("cst",)], writes=[(key, hp, c)])
                defer(lambda h, r=r: h.matmul(ps[PB_MISC][:, :], lhsT=ones_bf[:, :], rhs=sqq[:, r, :], start=True, stop=True),
                      [("sqq", r), ("ones",)], [("ps", PB_MISC)], age=1, then=post)
            yield from slab_gen(banks2, on)

        def on_v(c, bank):
            sc.add("dve", lambda h, c=c, bank=bank: h.tensor_copy(out=Vt[:, hp, 4 * c:4 * c + 4, :],
                                                                  in_=ps[bank][:, :].rearrange("p (t d) -> p t d", d=P)),
                   reads=[("ps", bank)], writes=[("Vt", hp, c)])
        yield from slab_gen(banks2, on_v, v_mode=True)
        flush(all_=True)

        sc.add("dve", lambda h: h.tensor_reduce(out=kms[:, :], in_=kT[:, hp, :].rearrange("p (j w) -> p j w", w=BLK), axis=AX.X, op=ALU.add),
               reads=[("kT", hp, c) for c in range(NPC)], writes=[("kms",)])
        sc.add("dve", lambda h: h.tensor_scalar(out=kmean_bf[:, :], in0=kms[:, :], scalar1=1.0 / BLK, scalar2=None, op0=ALU.mult),
               reads=[("kms",)], writes=[("kmean",)])

        def f_gate(h):
            ins = None
            for tq in range(8):
                t = 8 + tq
                ins = h.matmul(ps[PB_MISC][:, tq * 8:(tq + 1) * 8], lhsT=qT[:, hp, t * P:(t + 1) * P], rhs=kmean_bf[:, :],
                               start=True, stop=True)
            return ins

        def post_gate():
            sc.add("dve", lambda h: h.tensor_tensor(out=gm[:, :], in0=ps[PB_MISC][:, 0:64], in1=cand_f[:, :], op=ALU.add),
                   reads=[("ps", PB_MISC), ("cand",)], writes=[("gm",)])
            for tq in range(8):
                sc.add("dve", lambda h, tq=tq: h.max(out=top8[:, tq, :], in_=gm[:, tq * 8:(tq + 1) * 8]),
                       reads=[("gm",)], writes=[("top8", tq)])
                sc.add("dve", lambda h, tq=tq: h.tensor_scalar(out=gm[:, tq * 8:(tq + 1) * 8], in0=gm[:, tq * 8:(tq + 1) * 8],
                                                               scalar1=top8[:, tq, TOPK - 1:TOPK], scalar2=BIG, op0=ALU.is_ge, op1=ALU.mult),
                       reads=[("gm",), ("top8", tq)], writes=[("gm",)])
            sc.add("dve", lambda h: h.tensor_tensor(out=negm_pad[:, :, 0:8], in0=gm[:, :].rearrange("p (t j) -> p t j", j=8),
                                                    in1=fixc_f[:, :].rearrange("p (t j) -> p t j", j=8), op=ALU.add),
                   reads=[("gm",), ("fixc",)], writes=[("negm_pad",)])
        defer(f_gate, [("qT", hp, 2), ("qT", hp, 3), ("kmean",)], [("ps", PB_MISC)], age=1, then=post_gate)

        def on_za(c, bank):
            sc.add("act", lambda h, c=c, bank=bank: h.activation(out=zs[:, hp, c * 512:(c + 1) * 512], in_=ps[bank][:, :], func=AF.Copy),
                   reads=[("ps", bank)], writes=[("zs", hp, c)])
        yield from slab_gen(banks2, on_za)
        sc.add("act", lambda h: h.activation(out=zs[:, hp, :], in_=zs[:, hp, :], func=AF.Silu),
               reads=[("zs", hp, c) for c in range(NPC)], writes=[("zs", hp, c) for c in range(NPC)])

        def f_tr(h):
            ins = None
            for tq in range(8):
                ins = h.transpose(out=ps2_bf[:, tq * P:(tq + 1) * P], in_=negm_pad[:, tq, :], identity=ident_bf[:, :])
            return ins
        flush(all_=True)
        sc.add("pe", f_tr, reads=[("negm_pad",), ("ident_bf",)], writes=[("ps", PB_MISC)])
        sc.add("dve", lambda h: h.tensor_copy(out=negmT[:, hp, :], in_=ps2_bf[:, :]), reads=[("ps", PB_MISC)], writes=[("negmT", hp)])
        yield

    pending_den = [None]

    def attention(hh, g):
        hp = hh % 2
        ntile = [0]

        def pull():
            if g is not None:
                next(g, None)

        for c in range(NPC):
            nk = 4 * c + 4
            tiles = []
            for kc in range(nk):
                i = kc - 4 * c
                qoff = 0 if i < 0 else P * i
                tiles.append((kc, qoff, 512 - qoff))

            def qk_parts(kc, qoff, N, c=c):
                sb = sbanks()
                j = kc // 2
                q0 = 512 * c + qoff
                mms = [("qk",)]
                for b in (2 * c, 2 * c + 1):
                    if b >= 4 and j < b:
                        lo = max(b * BLK, q0)
                        hi = (b + 1) * BLK
                        if hi > lo:
                            mms.append(("sel", lo - q0, hi - q0, lo - 4 * BLK))
                if kc >= 4 * c:
                    mms.append(("tri",))

                def f(h, kc=kc, q0=q0, N=N, sb=sb, mms=mms, j=j):
                    ins = None
                    for n_, m in enumerate(mms):
                        st, sp_ = (n_ == 0), (n_ == len(mms) - 1)
                        if m[0] == "qk":
                            ins = h.matmul(ps[sb][:, 0:N], lhsT=kT[:, hp, kc * P:(kc + 1) * P], rhs=qT[:, hp, q0:q0 + N], start=st, stop=sp_)
                        elif m[0] == "sel":
                            ins = h.matmul(ps[sb][:, m[1]:m[2]], lhsT=selj_bf[:, j * P:(j + 1) * P],
                                           rhs=negmT[:, hp, m[3]:m[3] + (m[2] - m[1])], start=st, stop=sp_)
                        else:
                            ins = h.matmul(ps[sb][:, 0:P], lhsT=ident_bf[:, :], rhs=tri_bf[:, :], start=st, stop=sp_)
                    return ins
                reads = [("kT", hp, kc // 4), ("qT", hp, c), ("negmT", hp), ("selj",), ("ident_bf",), ("tri",)]
                pr = prr[0] % 3
                prr[0] += 1
                return f, reads, sb, pr, N

            def add_exp(sb, pr, N):
                sc.add("act", lambda h, sb=sb, N=N, pr=pr: h.activation(out=pT[:, pr, 0:N], in_=ps[sb][:, 0:N], func=AF.Exp,
                                                                        scale=scale, bias=nbias[:, 0:1]),
                       reads=[("ps", sb), ("nbias",)], writes=[("pT", pr)])

            def add_pool(pr, N, first=False):
                if first:
                    sc.add("pool", lambda h, pr=pr: h.tensor_copy(out=acc[:, :], in_=pT[:, pr, :]),
                           reads=[("pT", pr)], writes=[("acc",)])
                else:
                    sc.add("pool", lambda h, pr=pr, N=N: h.tensor_tensor(out=acc[:, 512 - N:512], in0=acc[:, 512 - N:512], in1=pT[:, pr, 0:N], op=ALU.add),
                           reads=[("pT", pr), ("acc",)], writes=[("acc",)])

            def pv_fn(kc, qoff, N, pr, nk=nk):
                def f(h, kc=kc, qoff=qoff, N=N, pr=pr):
                    return h.matmul(ps[PB_O][:, qoff:512], lhsT=Vt[:, hp, kc, :], rhs=pT[:, pr, 0:N], start=(kc == 0), stop=(kc == nk - 1))
                return f, [("pT", pr), ("Vt", hp, kc // 4)]

            qk = [None] * nk
            for n_ in range(min(2, nk)):
                qk[n_] = qk_parts(*tiles[n_])
                sc.add("pe", qk[n_][0], reads=qk[n_][1], writes=[("ps", qk[n_][2])])
                add_exp(qk[n_][2], qk[n_][3], qk[n_][4])
            if pending_den[0] is not None:
                pending_den[0]()
                pending_den[0] = None
            for n_ in range(min(2, nk)):
                add_pool(qk[n_][3], qk[n_][4], first=(n_ == 0))
            for n_ in range(nk):
                if ntile[0] % 8 != 7:
                    pull()
                fpv, rpv = pv_fn(*tiles[n_], qk[n_][3])
                if n_ + 2 < nk:
                    qk[n_ + 2] = qk_parts(*tiles[n_ + 2])
                    fq, rq_, sbq = qk[n_ + 2][0], qk[n_ + 2][1], qk[n_ + 2][2]

                    def fboth(h, fpv=fpv, fq=fq):
                        fpv(h)
                        return fq(h)
                    sc.add("pe", fboth, reads=rpv + rq_, writes=[("ps", PB_O), ("ps", sbq)])
                    add_exp(sbq, qk[n_ + 2][3], qk[n_ + 2][4])
                    add_pool(qk[n_ + 2][3], qk[n_ + 2][4])
                else:
                    sc.add("pe", fpv, reads=rpv, writes=[("ps", PB_O)])
                flush()
                ntile[0] += 1
            sc.add("act", lambda h: h.activation(out=at_t[:, 0, :], in_=ps[PB_O][:, :], func=AF.Copy),
                   reads=[("ps", PB_O)], writes=[("at",)])
            r = c % 2
            gah = cst[:, C_GA + hh:C_GA + hh + 1]

            def post_den(c=c, r=r, gah=gah):
                sc.add("dve", lambda h: h.reciprocal(out=rden[:, 0, :], in_=ps[PB_D][:, :]),
                       reads=[("ps", PB_D)], writes=[("rden",)])
                sc.add("dve", lambda h: h.tensor_tensor(out=at_t[:, 0, :], in0=at_t[:, 0, :], in1=rden[:, 0, :], op=ALU.mult),
                       reads=[("at",), ("rden",)], writes=[("at",)])
                sc.add("act", lambda h, r=r: h.activation(out=sqa[:, r, :], in_=at_t[:, 0, :], func=AF.Square),
                       reads=[("at",)], writes=[("sqa", r)])
                defer(stats_mm(sqa[:, r, :], NT + 4 * c), [("sqa", r), ("ones",)], [("ps", PB_STAT)], age=7)
                sc.add("dve", lambda h, c=c, gah=gah: h.scalar_tensor_tensor(
                    out=yT[:, CC + hh, c * 512:(c + 1) * 512], in0=at_t[:, 0, :], scalar=gah, in1=zs[:, hp, c * 512:(c + 1) * 512],
                    op0=ALU.mult, op1=ALU.mult),
                    reads=[("at",), ("zs", hp, c), ("cst",)], writes=[k_yT(CC + hh, c)], extra=xs_readers())
            def den_now(post_den=post_den):
                sc.add("pe", lambda h: h.matmul(ps[PB_D][:, :], lhsT=ones_f[:, :], rhs=acc[:, :], start=True, stop=True),
                       reads=[("acc",), ("ones_f",)], writes=[("ps", PB_D)])
                post_den()
            pending_den[0] = den_now

    g0 = proj_steps(0)
    for _ in g0:
        pass
    for hh in range(H):
        g = proj_steps(hh + 1) if hh + 1 < H else None
        attention(hh, g)
        if g is not None:
            for _ in g:
                pass
    if pending_den[0] is not None:
        pending_den[0]()
        pending_den[0] = None
    flush(all_=True)

    sc.add("act", lambda h: h.activation(out=stat_sb[:, 0:NT], in_=ps[PB_STAT][:, 0:NT], func=AF.Ln, scale=1.0 / (CC * P), bias=EPS),
           reads=[("ps", PB_STAT)], writes=[("stat", 0)])
    sc.add("act", lambda h: h.activation(out=stat_sb[:, NT:2 * NT], in_=ps[PB_STAT][:, NT:2 * NT], func=AF.Ln, scale=1.0 / (H * P), bias=EPS),
           reads=[("ps", PB_STAT)], writes=[("stat", 1)])
    sc.add("act", lambda h: h.activation(out=stat_sb[:, :], in_=stat_sb[:, :], func=AF.Exp, scale=-0.5),
           reads=[("stat", 0), ("stat", 1)], writes=[("stat", 0), ("stat", 1)])

    HK = [k_hT(k, c) for k in range(KC) for c in range(NPC)]
    hk_deps = set()
    for k_ in HK:
        hk_deps |= set(sc.readers.get(k_, ()))
    first_use = set()

    def hk(key):
        if key in first_use:
            return ()
        first_use.add(key)
        return list(hk_deps)
    banks3 = rr([0, 1, 2, 3, 4, 5])
    x_t = x_d.rearrange("(t p) d -> t p d", p=P)
    o_t = out_d.rearrange("(t p) d -> t p d", p=P)
    outs = []
    iters = [(n, t) for n in range(ND) for t in range(NT)]
    PF = XR - 1

    def issue_x(i):
        n, t = iters[i]
        ld("sp", xres[:, i % XR, :], x_t[t, :, n * 512:(n + 1) * 512], [("xres", i % XR)], extra=hk(("xres", i % XR)))

    for i in range(min(PF, len(iters))):
        issue_x(i)
    for it, (n, t) in enumerate(iters):
        wb = n % 2
        if t == 0:
            ld("pool", wo[wb][:, :, :].rearrange("p e c -> p (e c)"), wout_d[n, :, :], [("wo", wb)], extra=hk(("wo", wb)))
        if it + PF < len(iters):
            issue_x(it + PF)
        rx = it % XR
        r3 = it % 3
        r2 = it % 2
        b1 = banks3()
        b2 = banks3()

        def f(h, t=t, wb=wb, b1=b1, b2=b2):
            ins = None
            for e in range(KC):
                bank = b1 if e < CC else b2
                ee = e if e < CC else e - CC
                last = (CC - 1) if e < CC else (KC - CC - 1)
                ins = h.matmul(ps[bank][:, :], lhsT=yT[:, e, t * P:(t + 1) * P], rhs=wo[wb][:, e, :], start=(ee == 0), stop=(ee == last))
            return ins
        sc.add("pe", f, reads=[("wo", wb)] + [k_yT(e, t // 4) for e in range(KC)], writes=[("ps", b1), ("ps", b2)])
        sc.add("dve", lambda h, t=t, b1=b1, rx=rx, r2=r2: h.scalar_tensor_tensor(
            out=t1[:, r2, :], in0=ps[b1][:, :], scalar=stat_sb[:, t:t + 1], in1=xres[:, rx, :], op0=ALU.mult, op1=ALU.add),
            reads=[("ps", b1), ("stat", 0), ("xres", rx)], writes=[("t1", r2)], extra=hk(("t1", r2)))
        sc.add("dve", lambda h, t=t, b2=b2, r3=r3, r2=r2: h.scalar_tensor_tensor(
            out=ot[:, r3, :], in0=ps[b2][:, :], scalar=stat_sb[:, NT + t:NT + t + 1], in1=t1[:, r2, :], op0=ALU.mult, op1=ALU.add),
            reads=[("ps", b2), ("stat", 1), ("t1", r2)], writes=[("ot", r3)], extra=hk(("ot", r3)))
        outs.append(sc.add("act", lambda h, t=t, n=n, r3=r3: h.dma_start(out=o_t[t, :, n * 512:(n + 1) * 512], in_=ot[:, r3, :]),
                           reads=[("ot", r3)], writes=[], dma=True))

    for name in dbg:
        src = {"hT": hT, "yT": yT, "qT": qT, "kT": kT, "Vt": Vt, "zs": zs, "negmT": negmT, "stat": stat_sb}[name]
        shp = list(src.shape)
        dt_ = BF16 if src.dtype == BF16 else F32
        d = nc.dram_tensor("dbg_" + name, shp, dt_, kind="ExternalOutput").ap()
        keys = {"hT": HK, "yT": [k_yT(e, c) for e in range(KC) for c in range(NPC)], "qT": [("qT", hp_, c) for hp_ in range(2) for c in range(NPC)],
                "kT": [("kT", hp_, c) for hp_ in range(2) for c in range(NPC)], "Vt": [("Vt", hp_, c) for hp_ in range(2) for c in range(NPC)], "zs": [("zs", hp_, c) for hp_ in range(2) for c in range(NPC)],
                "negmT": [("negmT", 0), ("negmT", 1)], "stat": [("stat", 0), ("stat", 1)]}[name]
        full = tuple(slice(None) for _ in shp)
        outs.append(sc.add("sp", lambda h, d=d, src=src, full=full: h.dma_start(out=d[full], in_=src[full]), reads=keys, dma=True))

    sc.add("sp", None, extra=outs, name="final")

    return _emit(nc, sc)


def _emit(nc, sc):
    import contextlib
    with contextlib.ExitStack() as es:
        csem = {e: es.enter_context(nc.semaphore("c_" + e)) for e in ("pe", "act", "dve", "pool")}
        dsems = {e: [es.enter_context(nc.semaphore("d_%s%d" % (e, i))) for i in range(NDMA)] for e in ("sp", "pool", "act")}
        dsems["dve"] = dsems["pe"] = []
        sc.finalize(csem, dsems)
        block = es.enter_context(nc.Block())

        @block.tensor
        def _(h):
            sc.emit("pe", h)

        @block.scalar
        def _(h):
            sc.emit("act", h)

        @block.vector
        def _(h):
            sc.emit("dve", h)

        @block.gpsimd
        def _(h):
            sc.emit("pool", h)

        @block.sync
        def _(h):
            sc.emit("sp", h)
    return nc


def make_kst():
    k = np.zeros((P, NKST), np.float32)
    k[:, K_ID:K_ID + P] = np.eye(P, dtype=np.float32)
    kk = np.arange(P)[:, None]
    qq = np.arange(P)[None, :]
    k[:, K_TRI:K_TRI + P] = np.where(kk <= qq, 0.0, -BIG)
    for j in range(NBLK):
        k[j, K_SEL + j * P:K_SEL + (j + 1) * P] = 1.0
    cand = np.zeros((8, 8), np.float32)
    fix = np.zeros((8, 8), np.float32)
    for tq in range(8):
        own = (8 + tq) // 2
        for j in range(8):
            cand[tq, j] = 0.0 if j < own else -1e30
            fix[tq, j] = 0.0 if j == own else -BIG
    k[:, K_CAND:K_CAND + 64] = cand.reshape(1, 64)
    k[:, K_FIX:K_FIX + 64] = fix.reshape(1, 64)
    return k


def host_layout(x, norm_gain, w_in, conv_w, q_norm_gain, k_norm_gain, conv_out_gain, attn_out_gain, w_out):
    B, S_, D = x.shape
    KC = D // P
    CC = KC // 2
    H = KC // 2
    NCH = 4 * KC
    ND = D // 512
    f = np.float32
    w_in_r = np.ascontiguousarray(np.asarray(w_in[0], f).reshape(KC, P, NCH, P).transpose(2, 1, 0, 3)).reshape(NCH, P, KC * P)
    w_out_r = np.ascontiguousarray(np.asarray(w_out[0], f).reshape(KC, P, ND, 512).transpose(2, 1, 0, 3)).reshape(ND, P, KC * 512)
    cst = np.concatenate([
        np.asarray(norm_gain[0], f).reshape(KC, P).T,
        np.asarray(conv_w[0], f).reshape(3, CC, P).transpose(2, 1, 0).reshape(P, 3 * CC),
        np.asarray(conv_out_gain[0], f).reshape(CC, P).T,
        np.asarray(attn_out_gain[0], f).reshape(H, P).T,
        np.asarray(q_norm_gain[0], f).reshape(P, 1),
        np.asarray(k_norm_gain[0], f).reshape(P, 1),
    ], axis=1)
    cst = np.ascontiguousarray(cst, dtype=f)
    kst = make_kst()
    maps = []
    for b in range(B):
        xb = np.ascontiguousarray(np.asarray(x[b], f))
        maps.append({"xT": np.ascontiguousarray(xb.T), "x": xb, "w_in": w_in_r, "w_out": w_out_r, "cst": cst, "kst": kst})
    return maps


_NC_CACHE = {}


def kernel(x, norm_gain, w_in, conv_w, q_norm_gain, k_norm_gain, conv_out_gain, attn_out_gain, w_out):
    x = np.asarray(x)
    B, S_, D = x.shape
    assert S_ == S
    maps = host_layout(x, np.asarray(norm_gain), np.asarray(w_in), np.asarray(conv_w), np.asarray(q_norm_gain),
                       np.asarray(k_norm_gain), np.asarray(conv_out_gain), np.asarray(attn_out_gain), np.asarray(w_out))
    nc = build(D)
    res = run_bass_kernel_spmd(nc, maps, core_ids=list(range(B)))
    return np.stack([np.asarray(r["out"], np.float32) for r in res.results], axis=0)
```

```python
import numpy as np
import concourse.bass as bass
import concourse.mybir as mybir
from concourse.bass_utils import run_bass_kernel_spmd

F32 = mybir.dt.float32
BF16 = mybir.dt.bfloat16
AF = mybir.ActivationFunctionType
ALU = mybir.AluOpType
AX = mybir.AxisListType

P = 128
S = 2048
BLK = 256
NBLK = S // BLK
TOPK = 3
BIG = 32768.0
EPS = 1e-6
NPC = S // 512
NT = S // P
NSLOT = 4
NDMA = 8

K_ID, K_TRI, K_SEL, K_CAND, K_FIX = 0, 128, 256, 256 + 1024, 256 + 1024 + 64
NKST = K_FIX + 64


class Task:
    __slots__ = ("eng", "fn", "deps", "pos", "sig", "sigval", "dma", "dsem", "dval", "dprev", "name")

    def __init__(self, eng, fn, dma, name):
        self.eng = eng
        self.fn = fn
        self.dma = dma
        self.name = name
        self.deps = set()
        self.pos = -1
        self.sig = False
        self.sigval = 0
        self.dsem = None
        self.dval = 0
        self.dprev = None


class Sched:
    ENGS = ("pe", "act", "dve", "pool", "sp")

    def __init__(self):
        self.tasks = {e: [] for e in self.ENGS}
        self.last_w = {}
        self.readers = {}
        self.ndma = {e: 0 for e in self.ENGS}

    def add(self, eng, fn, reads=(), writes=(), dma=False, name="", extra=()):
        t = Task(eng, fn, dma, name)
        deps = set(extra)
        for k in reads:
            w = self.last_w.get(k)
            if w is not None:
                deps.add(w)
        for k in writes:
            w = self.last_w.get(k)
            if w is not None:
                deps.add(w)
            for r in self.readers.get(k, ()):
                deps.add(r)
        for k in reads:
            self.readers.setdefault(k, []).append(t)
        for k in writes:
            self.last_w[k] = t
            self.readers[k] = []
        deps.discard(t)
        t.pos = len(self.tasks[eng])
        self.tasks[eng].append(t)
        best = {}
        keep = []
        for d in deps:
            if d.dma:
                keep.append(d)
                continue
            if d.eng == "pe" and eng == "pe" and not dma:
                continue
            b = best.get(d.eng)
            if b is None or d.pos > b.pos:
                best[d.eng] = d
        keep.extend(best.values())
        t.deps = keep
        for d in keep:
            d.sig = True
        if dma:
            t.sig = True
        return t

    def last(self, eng):
        ts = self.tasks[eng]
        return ts[-1] if ts else None

    def barrier(self, engs):
        lasts = [self.last(e) for e in self.ENGS]
        lasts = [t for t in lasts if t is not None]
        for e in engs:
            self.add(e, None, extra=[t for t in lasts if t.eng != e], name="barrier")

    def finalize(self, csem, dsems):
        for e in self.ENGS:
            cnt = 0
            nd = 0
            for t in self.tasks[e]:
                if t.dma:
                    t.dsem = dsems[e][nd % NDMA]
                    t.dval = 16 * (nd // NDMA + 1)
                    nd += 1
                elif t.sig and t.fn is not None:
                    cnt += 1
                    t.sigval = cnt
                elif t.sig:
                    raise RuntimeError("no-op task used as dependency")
        self.csem = csem

    def emit(self, e, h):
        waited = {}

        def wait(sem, key, val):
            if waited.get(key, 0) < val:
                h.wait_ge(sem, val)
                waited[key] = val

        nd = 0
        for t in self.tasks[e]:
            for d in t.deps:
                if d.dma:
                    wait(d.dsem, ("d", d.eng, id(d.dsem)), d.dval)
                else:
                    wait(self.csem[d.eng], ("c", d.eng), d.sigval)
            if t.dma:
                if t.dval > 16:
                    wait(t.dsem, ("d", e, id(t.dsem)), t.dval - 16)
                ins = t.fn(h)
                ins.then_inc(t.dsem, 16)
                nd += 1
            elif t.fn is not None:
                ins = t.fn(h)
                if t.sig:
                    ins.then_inc(self.csem[e], 1)


def build(D, dbg=()):
    KC = D // P
    CC = KC // 2
    H = KC // 2
    NCH = 4 * KC
    ND = D // 512
    NCST = KC + 3 * CC + CC + H + 2
    C_GX, C_CW, C_GC, C_GA, C_GQ, C_GK = 0, KC, KC + 3 * CC, KC + 4 * CC, KC + 4 * CC + H, KC + 4 * CC + H + 1

    nc = bass.Bass("TRN2", target_bir_lowering=False)
    xT_d = nc.dram_tensor("xT", [D, S], F32, kind="ExternalInput").ap()
    x_d = nc.dram_tensor("x", [S, D], F32, kind="ExternalInput").ap()
    win_d = nc.dram_tensor("w_in", [NCH, P, KC * P], F32, kind="ExternalInput").ap()
    wout_d = nc.dram_tensor("w_out", [ND, P, KC * 512], F32, kind="ExternalInput").ap()
    cst_d = nc.dram_tensor("cst", [P, NCST], F32, kind="ExternalInput").ap()
    kst_d = nc.dram_tensor("kst", [P, NKST], F32, kind="ExternalInput").ap()
    out_d = nc.dram_tensor("out", [S, D], F32, kind="ExternalOutput").ap()
    dbg_d = {}

    SB_BASE, SB_END = 16512, 229344
    off = [SB_BASE]

    def alloc(name, shape, dt, at=None):
        nbytes = int(np.prod(shape[1:])) * (4 if dt == F32 else 2)
        if at is None:
            at = off[0]
            off[0] = (at + nbytes + 31) // 32 * 32
        return nc.alloc_sbuf_tensor_at(name, list(shape), dt, offset=at)

    HT_OFF = SB_BASE
    hT = alloc("hT", [P, KC, S], BF16)
    YT_OFF = off[0]
    yT = alloc("yT", [P, KC, S], BF16)
    wring = alloc("wring", [P, NSLOT, KC * P], BF16)
    cst = alloc("cst_sb", [P, NCST], F32)
    ident_bf = alloc("ident_bf", [P, P], BF16)
    tri_bf = alloc("tri_bf", [P, P], BF16)
    ones_bf = alloc("ones_bf", [P, P], BF16)
    selj_bf = alloc("selj_bf", [P, NBLK * P], BF16)
    ident_f = alloc("ident_f", [P, P], F32)
    cand_f = alloc("cand_f", [P, 64], F32)
    fixc_f = alloc("fixc_f", [P, 64], F32)
    nbias = alloc("nbias", [P, 1], F32)
    gqs = alloc("gqs", [P, 2], F32)
    stat_sb = alloc("stat_sb", [P, 2 * NT], F32)
    kms = alloc("kms", [P, NBLK], F32)
    kmean_bf = alloc("kmean_bf", [P, NBLK], BF16)
    ones_f = alloc("ones_f", [P, P], F32)
    SCR = off[0]
    off[0] = SCR
    hs = alloc("hs", [P, NPC, 512], F32)
    u = alloc("u", [P, 2 + S + 6], F32)
    a_t = alloc("a_t", [P, NPC, 512], F32)
    y_t = alloc("y_t", [P, 2, 512], F32)
    sqc = alloc("sqc", [P, 2, 512], BF16)
    sz1 = alloc("sz1", [P, NPC, 512], F32)
    sq0 = alloc("sq0", [P, 2, 4, 512], BF16)
    rstd = alloc("rstd", [P, 2, 512], F32)
    prm = alloc("prm", [P, P], F32)
    onesf = alloc("onesf", [P, P], F32)
    END1 = off[0]
    off[0] = SCR
    qf = alloc("qf", [P, 2, 512], F32)
    sqq = alloc("sqq", [P, 2, 512], BF16)
    rq = alloc("rq", [P, 1, 512], F32)
    qT = alloc("qT", [P, 2, S], BF16)
    kT = alloc("kT", [P, 2, S], BF16)
    Vt = alloc("Vt", [P, 2, NT, P], BF16)
    zs = alloc("zs", [P, 2, S], BF16)
    pT = alloc("pT", [P, 3, 512], BF16)
    rden = alloc("rden", [P, 1, 512], F32)
    acc = alloc("acc", [P, 512], F32)
    at_t = alloc("at_t", [P, 1, 512], F32)
    sqa = alloc("sqa", [P, 2, 512], BF16)
    gm = alloc("gm", [P, 64], F32)
    top8 = alloc("top8", [P, 8, 8], F32)
    negm_pad = alloc("negm_pad", [P, 8, P], BF16)
    negmT = alloc("negmT", [P, 2, 8 * P], BF16)
    END2 = off[0]
    assert max(END1, END2) <= SB_END, (END1, END2)
    WO_BYTES = KC * 512 * 2
    wo = [alloc("wo%d" % i, [P, KC, 512], BF16, at=HT_OFF + i * WO_BYTES) for i in range(2)]
    p3 = HT_OFF + 2 * WO_BYTES
    XR = 4 if KC >= 16 else 2
    xres = alloc("xres", [P, XR, 512], F32, at=p3)
    t1 = alloc("t1", [P, 2, 512], F32, at=p3 + XR * 2048)
    ot = alloc("ot", [P, 3, 512], F32, at=p3 + XR * 2048 + 4096)
    assert p3 + XR * 2048 + 4096 + 6144 <= HT_OFF + KC * S * 2
    xs = alloc("xs", [P, 2, KC, 512], F32, at=YT_OFF)
    assert 2 * KC * 512 * 4 <= KC * S * 2

    ps = [nc.alloc_psum_tensor("ps%d" % i, [P, 512], F32) for i in range(8)]
    ps2_bf = ps[2].bitcast(BF16)
    PB_MISC, PB_STAT = 2, 7

    sc = Sched()

    def k_hT(k, c):
        return ("hT", k, c)

    def k_yT(e, c):
        return ("yT", e, c)

    def ld(eng, out_ap, in_ap, writes, reads=(), extra=()):
        return sc.add(eng, lambda h, o=out_ap, i=in_ap: h.dma_start(out=o, in_=i), reads=reads, writes=writes, dma=True, extra=extra)

    ld("sp", cst[:, :], cst_d[:, :], [("cst",)])
    ld("sp", ident_f[:, :], kst_d[:, K_ID:K_ID + P], [("ident_f",)])
    ld("sp", cand_f[:, :], kst_d[:, K_CAND:K_CAND + 64], [("cand",)])
    ld("sp", fixc_f[:, :], kst_d[:, K_FIX:K_FIX + 64], [("fixc",)])
    ld("pool", ident_bf[:, :], kst_d[:, K_ID:K_ID + P], [("ident_bf",)])
    ld("pool", tri_bf[:, :], kst_d[:, K_TRI:K_TRI + P], [("tri",)])
    ld("pool", selj_bf[:, :], kst_d[:, K_SEL:K_SEL + NBLK * P], [("selj",)])
    sc.add("dve", lambda h: h.memset(ones_bf[:, :], 1.0), writes=[("ones",)])
    sc.add("dve", lambda h: h.memset(onesf[:, :], 1.0), writes=[("onesf",)])
    sc.add("dve", lambda h: h.memset(ones_f[:, :], 1.0), writes=[("ones_f",)])
    sc.add("dve", lambda h: h.memset(u[:, 0:2], 0.0), writes=[("u", -1)])

    seq = []
    for cc in range(CC):
        seq += [cc, 2 * CC + cc, 3 * CC + cc, CC + cc]
    for hh in range(H):
        seq += [4 * CC + 3 * H + hh, 4 * CC + hh, 4 * CC + H + hh, 4 * CC + 2 * H + hh]
    wnext = [0]

    def issue_w(extra=()):
        i = wnext[0]
        if i >= len(seq):
            return
        wnext[0] += 1
        slot = i % NSLOT
        ld("pool", wring[:, slot, :], win_d[seq[i], :, :], [("w", slot)], extra=extra)

    issue_w()

    sc.add("dve", lambda h: h.tensor_tensor(out=gqs[:, 0:1], in0=cst[:, C_GQ:C_GQ + 1], in1=cst[:, C_GK:C_GK + 1], op=ALU.mult),
           reads=[("cst",)], writes=[("gqs",)])
    sc.add("dve", lambda h: h.tensor_scalar(out=prm[:, :], in0=ident_f[:, :], scalar1=gqs[:, 0:1], scalar2=None, op0=ALU.mult),
           reads=[("gqs",), ("ident_f",)], writes=[("prm",)])
    sc.add("pe", lambda h: h.matmul(ps[5][:, 0:P], lhsT=onesf[:, :], rhs=prm[:, :], start=True, stop=True),
           reads=[("prm",), ("onesf",)], writes=[("ps", 5)])
    sc.add("dve", lambda h: h.tensor_reduce(out=gqs[:, 1:2], in_=ps[5][:, 0:P], axis=AX.X, op=ALU.max, apply_absolute_value=True),
           reads=[("ps", 5)], writes=[("gqs2",)])
    sc.add("dve", lambda h: h.tensor_scalar(out=nbias[:, :], in0=gqs[:, 1:2], scalar1=-float(np.sqrt(P)), scalar2=None, op0=ALU.mult),
           reads=[("gqs2",)], writes=[("nbias",)])

    xT_r = xT_d.rearrange("(k p) t -> p k t", p=P)
    KG = KC // 4
    def prologue_chunk(c):
        half = c % 2
        xl = []
        for g in range(KG):
            xl.append(ld("sp", xs[:, half, 4 * g:4 * g + 4, :], xT_r[:, 4 * g:4 * g + 4, c * 512:(c + 1) * 512],
                         [("xs", half, g)]))
        if c == 0:
            for _ in range(NSLOT - 1):
                issue_w(extra=xl)
        for g in range(KG):
            b = (c * KG + g) % 2
            sc.add("act", lambda h, b=b, half=half, g=g: h.activation(out=sq0[:, b, :, :], in_=xs[:, half, 4 * g:4 * g + 4, :], func=AF.Square),
                   reads=[("xs", half, g)], writes=[("sq0", b)])

            def f(h, b=b, g=g):
                ins = None
                for kk in range(4):
                    ins = h.matmul(ps[6][:, :], lhsT=ones_bf[:, :], rhs=sq0[:, b, kk, :],
                                   start=(g == 0 and kk == 0), stop=(g == KG - 1 and kk == 3))
                return ins
            sc.add("pe", f, reads=[("sq0", b), ("ones",)], writes=[("ps", 6)])
        r = c % 2
        sc.add("act", lambda h, r=r: h.activation(out=rstd[:, r, :], in_=ps[6][:, :], func=AF.Ln, scale=1.0 / D, bias=EPS),
               reads=[("ps", 6)], writes=[("rstd", r)])
        sc.add("act", lambda h, r=r: h.activation(out=rstd[:, r, :], in_=rstd[:, r, :], func=AF.Exp, scale=-0.5),
               reads=[("rstd", r)], writes=[("rstd", r)])
        for k in range(KC):
            sc.add("dve", lambda h, k=k, c=c, half=half, r=r: h.scalar_tensor_tensor(
                out=hT[:, k, c * 512:(c + 1) * 512], in0=xs[:, half, k, :], scalar=cst[:, C_GX + k:C_GX + k + 1],
                in1=rstd[:, r, :], op0=ALU.mult, op1=ALU.mult),
                reads=[("xs", half, k // 4), ("rstd", r), ("cst",)], writes=[k_hT(k, c)])

    def xs_readers():
        d = set()
        for half_ in range(2):
            for g_ in range(KG):
                d |= set(sc.readers.get(("xs", half_, g_), ()))
                w_ = sc.last_w.get(("xs", half_, g_))
                if w_ is not None:
                    d.add(w_)
        return list(d)

    pend = []

    def defer(fn, reads, writes, age=1, then=None):
        pend.append([age, fn, reads, writes, then])

    def flush(all_=False):
        keep = []
        todo = list(pend)
        pend[:] = []
        for it in todo:
            if it[0] <= 0 or all_:
                sc.add("pe", it[1], reads=it[2], writes=it[3])
                if it[4] is not None:
                    it[4]()
            else:
                it[0] -= 1
                keep.append(it)
        pend[:] = keep + pend

    widx = [0]

    def slab_gen(banks, on_group, v_mode=False):
        i = widx[0]
        widx[0] += 1
        slot = i % NSLOT
        KH = KC // 2
        for c in range(NPC):
            bank = banks()
            for half in range(2):
                if not v_mode:
                    def f(h, c=c, bank=bank, slot=slot, half=half):
                        ins = None
                        for k in range(half * KH, (half + 1) * KH):
                            ins = h.matmul(ps[bank][:, :], lhsT=wring[:, slot, k * P:(k + 1) * P], rhs=hT[:, k, c * 512:(c + 1) * 512],
                                           start=(k == 0), stop=(k == KC - 1))
                        return ins
                else:
                    def f(h, c=c, bank=bank, slot=slot, half=half):
                        ins = None
                        for tt in range(2 * half, 2 * half + 2):
                            t = 4 * c + tt
                            for k in range(KC):
                                ins = h.matmul(ps[bank][:, tt * P:(tt + 1) * P], lhsT=hT[:, k, t * P:(t + 1) * P],
                                               rhs=wring[:, slot, k * P:(k + 1) * P], start=(k == 0), stop=(k == KC - 1))
                        return ins
                sc.add("pe", f, reads=[("w", slot)] + [k_hT(k, c) for k in range(KC)], writes=[("ps", bank)])
                if half == 0:
                    yield
            flush()
            on_group(c, bank)
            if c == NPC - 1:
                issue_w()
            yield

    def slab(banks, on_group, v_mode=False):
        for _ in slab_gen(banks, on_group, v_mode):
            pass

    def rr(banks_list):
        st = [0]

        def nxt():
            b = banks_list[st[0] % len(banks_list)]
            st[0] += 1
            return b
        return nxt

    stat_started = [False]

    def stats_mm(src, col0):
        def f(h, src=src, col0=col0):
            ins = None
            for tt in range(4):
                first = not stat_started[0]
                stat_started[0] = True
                ins = h.matmul(ps[PB_STAT][:, col0 + tt:col0 + tt + 1], lhsT=src[:, tt * P:(tt + 1) * P], rhs=ones_bf[:, 0:1],
                               start=first, stop=True, skip_group_check=True)
            return ins
        return f

    banks1 = rr([0, 1, 2, 3, 4, 5])
    for cc in range(CC):
        def on_h(c, bank):
            sc.add("act", lambda h, c=c, bank=bank: h.activation(out=hs[:, c, :], in_=ps[bank][:, :], func=AF.Copy),
                   reads=[("ps", bank)], writes=[("hs", c)])

        def on_c(c, bank, cc=cc):
            sc.add("dve", lambda h, c=c, bank=bank: h.tensor_tensor(out=u[:, 2 + c * 512:2 + (c + 1) * 512], in0=ps[bank][:, :],
                                                                    in1=hs[:, c, :], op=ALU.mult),
                   reads=[("ps", bank), ("hs", c)], writes=[("u", c)])
            r = c
            w0 = cst[:, C_CW + 3 * cc + 0:C_CW + 3 * cc + 1]
            w1 = cst[:, C_CW + 3 * cc + 1:C_CW + 3 * cc + 2]
            w2 = cst[:, C_CW + 3 * cc + 2:C_CW + 3 * cc + 3]
            sc.add("act", lambda h, c=c, r=r, w2=w2: h.activation(out=a_t[:, r, :], in_=u[:, 2 + c * 512:2 + (c + 1) * 512],
                                                                   func=AF.Identity, scale=w2),
                   reads=[("u", c), ("cst",)], writes=[("a", r)])
            sc.add("dve", lambda h, c=c, r=r, w1=w1: h.scalar_tensor_tensor(out=a_t[:, r, :], in0=u[:, 1 + c * 512:1 + (c + 1) * 512],
                                                                            scalar=w1, in1=a_t[:, r, :], op0=ALU.mult, op1=ALU.add),
                   reads=[("u", c), ("u", c - 1), ("a", r)], writes=[("a", r)])
            sc.add("dve", lambda h, c=c, r=r, w0=w0: h.scalar_tensor_tensor(out=a_t[:, r, :], in0=u[:, c * 512:(c + 1) * 512],
                                                                            scalar=w0, in1=a_t[:, r, :], op0=ALU.mult, op1=ALU.add),
                   reads=[("u", c), ("u", c - 1), ("a", r)], writes=[("a", r)])

        def on_z(c, bank):
            sc.add("act", lambda h, c=c, bank=bank: h.activation(out=sz1[:, c, :], in_=ps[bank][:, :], func=AF.Silu),
                   reads=[("ps", bank)], writes=[("sz1", c)])

        def on_b(c, bank, cc=cc):
            r = c % 2
            sc.add("dve", lambda h, r=r, c=c, bank=bank: h.tensor_tensor(out=y_t[:, r, :], in0=ps[bank][:, :], in1=a_t[:, c, :], op=ALU.mult),
                   reads=[("ps", bank), ("a", c)], writes=[("y", r)])
            sc.add("act", lambda h, r=r: h.activation(out=sqc[:, r, :], in_=y_t[:, r, :], func=AF.Square),
                   reads=[("y", r)], writes=[("sqc", r)])
            defer(stats_mm(sqc[:, r, :], 4 * c), [("sqc", r), ("ones",)], [("ps", PB_STAT)], age=1)
            gcc = cst[:, C_GC + cc:C_GC + cc + 1]
            sc.add("dve", lambda h, r=r, c=c, cc=cc, gcc=gcc: h.scalar_tensor_tensor(
                out=yT[:, cc, c * 512:(c + 1) * 512], in0=y_t[:, r, :], scalar=gcc, in1=sz1[:, c, :], op0=ALU.mult, op1=ALU.mult),
                reads=[("y", r), ("sz1", c), ("cst",)], writes=[k_yT(cc, c)], extra=xs_readers())

        if cc == 0:
            g_h = slab_gen(banks1, on_h)
            g_c = slab_gen(banks1, on_c)
            prologue_chunk(0)
            for c in range(NPC):
                if c + 1 < NPC:
                    prologue_chunk(c + 1)
                next(g_h, None)
                next(g_h, None)
                next(g_c, None)
                next(g_c, None)
            for _ in g_h:
                pass
            for _ in g_c:
                pass
        else:
            slab(banks1, on_h)
            slab(banks1, on_c)
        slab(banks1, on_z)
        slab(banks1, on_b)
    flush(all_=True)

    if "p1" in dbg:
        outs = []
        for name, src in (("hs", hs), ("u", u), ("a_t", a_t), ("y_t", y_t), ("sz1", sz1), ("hT", hT), ("yT", yT), ("cst", cst)):
            shp = list(src.shape)
            d = nc.dram_tensor("dbg_" + name, shp, src.dtype, kind="ExternalOutput").ap()
            full = tuple(slice(None) for _ in shp)
            sc.barrier(["sp"])
            outs.append(sc.add("sp", lambda h, d=d, src=src, full=full: h.dma_start(out=d[full], in_=src[full]), dma=True))
        sc.add("sp", None, extra=outs, name="final")
        return _emit(nc, sc)
    sc.barrier(["act", "dve"])
    sc.add("dve", lambda h: h.memset(negm_pad[:, :, :], 0.0), writes=[("negm_pad",)])

    banks2 = rr([0, 1])
    sbanks = rr([3, 4])
    PB_O, PB_D = 5, 6
    prr = [0]
    scale = float(P) ** -0.5

    def proj_steps(hh):
        hp = hh % 2
        def on_za(c, bank):
            sc.add("act", lambda h, c=c, bank=bank: h.activation(out=zs[:, hp, c * 512:(c + 1) * 512], in_=ps[bank][:, :], func=AF.Copy),
                   reads=[("ps", bank)], writes=[("zs", hp, c)])
        yield from slab_gen(banks2, on_za)
        sc.add("act", lambda h: h.activation(out=zs[:, hp, :], in_=zs[:, hp, :], func=AF.Silu),
               reads=[("zs", hp, c) for c in range(NPC)], writes=[("zs", hp, c) for c in range(NPC)])

        for which, dst, gcol in ((0, qT, C_GQ), (1, kT, C_GK)):
            key = "qT" if which == 0 else "kT"

            def on(c, bank, dst=dst, gcol=gcol, key=key):
                r = c % 2
                sc.add("act", lambda h, r=r, bank=bank: h.activation(out=qf[:, r, :], in_=ps[bank][:, :], func=AF.Copy),
                       reads=[("ps", bank)], writes=[("qf", r)])
                sc.add("act", lambda h, r=r, bank=bank: h.activation(out=sqq[:, r, :], in_=ps[bank][:, :], func=AF.Square),
                       reads=[("ps", bank)], writes=[("sqq", r)])

                def post(r=r, c=c, dst=dst, gcol=gcol, key=key):
                    sc.add("act", lambda h, r=r: h.activation(out=rq[:, 0, :], in_=ps[PB_MISC][:, :], func=AF.Ln, scale=1.0 / P, bias=EPS),
                           reads=[("ps", PB_MISC)], writes=[("rq",)])
                    sc.add("act", lambda h, r=r: h.activation(out=rq[:, 0, :], in_=rq[:, 0, :], func=AF.Exp, scale=-0.5),
                           reads=[("rq",)], writes=[("rq",)])
                    sc.add("dve", lambda h, r=r, c=c, dst=dst, gcol=gcol: h.scalar_tensor_tensor(
                        out=dst[:, hp, c * 512:(c + 1) * 512], in0=qf[:, r, :], scalar=cst[:, gcol:gcol + 1], in1=rq[:, 0, :],
                        op0=ALU.mult, op1=ALU.mult),
                        reads=[("qf", r), ("rq",), ("cst",)], writes=[(key, hp, c)])
                defer(lambda h, r=r: h.matmul(ps[PB_MISC][:, :], lhsT=ones_bf[:, :], rhs=sqq[:, r, :], start=True, stop=True),
                      [("sqq", r), ("ones",)], [("ps", PB_MISC)], age=1, then=post)
            yield from slab_gen(banks2, on)

        def on_v(c, bank):
            sc.add("dve", lambda h, c=c, bank=bank: h.tensor_copy(out=Vt[:, hp, 4 * c:4 * c + 4, :],
                                                                  in_=ps[bank][:, :].rearrange("p (t d) -> p t d", d=P)),
                   reads=[("ps", bank)], writes=[("Vt", hp, c)])
        yield from slab_gen(banks2, on_v, v_mode=True)
        flush(all_=True)

        sc.add("dve", lambda h: h.tensor_reduce(out=kms[:, :], in_=kT[:, hp, :].rearrange("p (j w) -> p j w", w=BLK), axis=AX.X, op=ALU.add),
               reads=[("kT", hp, c) for c in range(NPC)], writes=[("kms",)])
        sc.add("dve", lambda h: h.tensor_scalar(out=kmean_bf[:, :], in0=kms[:, :], scalar1=1.0 / BLK, scalar2=None, op0=ALU.mult),
               reads=[("kms",)], writes=[("kmean",)])

        def f_gate(h):
            ins = None
            for tq in range(8):
                t = 8 + tq
                ins = h.matmul(ps[PB_MISC][:, tq * 8:(tq + 1) * 8], lhsT=qT[:, hp, t * P:(t + 1) * P], rhs=kmean_bf[:, :],
                               start=True, stop=True)
            return ins

        def post_gate():
            sc.add("dve", lambda h: h.tensor_tensor(out=gm[:, :], in0=ps[PB_MISC][:, 0:64], in1=cand_f[:, :], op=ALU.add),
                   reads=[("ps", PB_MISC), ("cand",)], writes=[("gm",)])
            for tq in range(8):
                sc.add("dve", lambda h, tq=tq: h.max(out=top8[:, tq, :], in_=gm[:, tq * 8:(tq + 1) * 8]),
                       reads=[("gm",)], writes=[("top8", tq)])
                sc.add("dve", lambda h, tq=tq: h.tensor_scalar(out=gm[:, tq * 8:(tq + 1) * 8], in0=gm[:, tq * 8:(tq + 1) * 8],
                                                               scalar1=top8[:, tq, TOPK - 1:TOPK], scalar2=BIG, op0=ALU.is_ge, op1=ALU.mult),
                       reads=[("gm",), ("top8", tq)], writes=[("gm",)])
            sc.add("dve", lambda h: h.tensor_tensor(out=negm_pad[:, :, 0:8], in0=gm[:, :].rearrange("p (t j) -> p t j", j=8),
                                                    in1=fixc_f[:, :].rearrange("p (t j) -> p t j", j=8), op=ALU.add),
                   reads=[("gm",), ("fixc",)], writes=[("negm_pad",)])
        defer(f_gate, [("qT", hp, 2), ("qT", hp, 3), ("kmean",)], [("ps", PB_MISC)], age=1, then=post_gate)

        def f_tr(h):
            ins = None
            for tq in range(8):
                ins = h.transpose(out=ps2_bf[:, tq * P:(tq + 1) * P], in_=negm_pad[:, tq, :], identity=ident_bf[:, :])
            return ins
        flush(all_=True)
        sc.add("pe", f_tr, reads=[("negm_pad",), ("ident_bf",)], writes=[("ps", PB_MISC)])
        sc.add("dve", lambda h: h.tensor_copy(out=negmT[:, hp, :], in_=ps2_bf[:, :]), reads=[("ps", PB_MISC)], writes=[("negmT", hp)])
        yield

    pending_den = [None]

    def attention(hh, g):
        hp = hh % 2
        ntile = [0]

        def pull():
            if g is not None:
                next(g, None)

        for c in range(NPC):
            nk = 4 * c + 4
            tiles = []
            for kc in range(nk):
                i = kc - 4 * c
                qoff = 0 if i < 0 else P * i
                tiles.append((kc, qoff, 512 - qoff))

            def qk_parts(kc, qoff, N, c=c):
                sb = sbanks()
                j = kc // 2
                q0 = 512 * c + qoff
                mms = [("qk",)]
                for b in (2 * c, 2 * c + 1):
                    if b >= 4 and j < b:
                        lo = max(b * BLK, q0)
                        hi = (b + 1) * BLK
                        if hi > lo:
                            mms.append(("sel", lo - q0, hi - q0, lo - 4 * BLK))
                if kc >= 4 * c:
                    mms.append(("tri",))

                def f(h, kc=kc, q0=q0, N=N, sb=sb, mms=mms, j=j):
                    ins = None
                    for n_, m in enumerate(mms):
                        st, sp_ = (n_ == 0), (n_ == len(mms) - 1)
                        if m[0] == "qk":
                            ins = h.matmul(ps[sb][:, 0:N], lhsT=kT[:, hp, kc * P:(kc + 1) * P], rhs=qT[:, hp, q0:q0 + N], start=st, stop=sp_)
                        elif m[0] == "sel":
                            ins = h.matmul(ps[sb][:, m[1]:m[2]], lhsT=selj_bf[:, j * P:(j + 1) * P],
                                           rhs=negmT[:, hp, m[3]:m[3] + (m[2] - m[1])], start=st, stop=sp_)
                        else:
                            ins = h.matmul(ps[sb][:, 0:P], lhsT=ident_bf[:, :], rhs=tri_bf[:, :], start=st, stop=sp_)
                    return ins
                reads = [("kT", hp, kc // 4), ("qT", hp, c), ("negmT", hp), ("selj",), ("ident_bf",), ("tri",)]
                pr = prr[0] % 3
                prr[0] += 1
                return f, reads, sb, pr, N

            def add_exp(sb, pr, N):
                sc.add("act", lambda h, sb=sb, N=N, pr=pr: h.activation(out=pT[:, pr, 0:N], in_=ps[sb][:, 0:N], func=AF.Exp,
                                                                        scale=scale, bias=nbias[:, 0:1]),
                       reads=[("ps", sb), ("nbias",)], writes=[("pT", pr)])

            def add_pool(pr, N, first=False):
                if first:
                    sc.add("pool", lambda h, pr=pr: h.tensor_copy(out=acc[:, :], in_=pT[:, pr, :]),
                           reads=[("pT", pr)], writes=[("acc",)])
                else:
                    sc.add("pool", lambda h, pr=pr, N=N: h.tensor_tensor(out=acc[:, 512 - N:512], in0=acc[:, 512 - N:512], in1=pT[:, pr, 0:N], op=ALU.add),
                           reads=[("pT", pr), ("acc",)], writes=[("acc",)])

            def pv_fn(kc, qoff, N, pr, nk=nk):
                def f(h, kc=kc, qoff=qoff, N=N, pr=pr):
                    return h.matmul(ps[PB_O][:, qoff:512], lhsT=Vt[:, hp, kc, :], rhs=pT[:, pr, 0:N], start=(kc == 0), stop=(kc == nk - 1))
                return f, [("pT", pr), ("Vt", hp, kc // 4)]

            qk = [None] * nk
            for n_ in range(min(2, nk)):
                qk[n_] = qk_parts(*tiles[n_])
                sc.add("pe", qk[n_][0], reads=qk[n_][1], writes=[("ps", qk[n_][2])])
                add_exp(qk[n_][2], qk[n_][3], qk[n_][4])
            if pending_den[0] is not None:
                pending_den[0]()
                pending_den[0] = None
            for n_ in range(min(2, nk)):
                add_pool(qk[n_][3], qk[n_][4], first=(n_ == 0))
            for n_ in range(nk):
                if ntile[0] % 8 != 7:
                    pull()
                fpv, rpv = pv_fn(*tiles[n_], qk[n_][3])
                if n_ + 2 < nk:
                    qk[n_ + 2] = qk_parts(*tiles[n_ + 2])
                    fq, rq_, sbq = qk[n_ + 2][0], qk[n_ + 2][1], qk[n_ + 2][2]

                    def fboth(h, fpv=fpv, fq=fq):
                        fpv(h)
                        return fq(h)
                    sc.add("pe", fboth, reads=rpv + rq_, writes=[("ps", PB_O), ("ps", sbq)])
                    add_exp(sbq, qk[n_ + 2][3], qk[n_ + 2][4])
                    add_pool(qk[n_ + 2][3], qk[n_ + 2][4])
                else:
                    sc.add("pe", fpv, reads=rpv, writes=[("ps", PB_O)])
                flush()
                ntile[0] += 1
            sc.add("act", lambda h: h.activation(out=at_t[:, 0, :], in_=ps[PB_O][:, :], func=AF.Copy),
                   reads=[("ps", PB_O)], writes=[("at",)])
            r = c % 2
            gah = cst[:, C_GA + hh:C_GA + hh + 1]

            def post_den(c=c, r=r, gah=gah):
                sc.add("dve", lambda h: h.reciprocal(out=rden[:, 0, :], in_=ps[PB_D][:, :]),
                       reads=[("ps", PB_D)], writes=[("rden",)])
                sc.add("dve", lambda h: h.tensor_tensor(out=at_t[:, 0, :], in0=at_t[:, 0, :], in1=rden[:, 0, :], op=ALU.mult),
                       reads=[("at",), ("rden",)], writes=[("at",)])
                sc.add("act", lambda h, r=r: h.activation(out=sqa[:, r, :], in_=at_t[:, 0, :], func=AF.Square),
                       reads=[("at",)], writes=[("sqa", r)])
                defer(stats_mm(sqa[:, r, :], NT + 4 * c), [("sqa", r), ("ones",)], [("ps", PB_STAT)], age=7)
                sc.add("dve", lambda h, c=c, gah=gah: h.scalar_tensor_tensor(
                    out=yT[:, CC + hh, c * 512:(c + 1) * 512], in0=at_t[:, 0, :], scalar=gah, in1=zs[:, hp, c * 512:(c + 1) * 512],
                    op0=ALU.mult, op1=ALU.mult),
                    reads=[("at",), ("zs", hp, c), ("cst",)], writes=[k_yT(CC + hh, c)], extra=xs_readers())
            def den_now(post_den=post_den):
                sc.add("pe", lambda h: h.matmul(ps[PB_D][:, :], lhsT=ones_f[:, :], rhs=acc[:, :], start=True, stop=True),
                       reads=[("acc",), ("ones_f",)], writes=[("ps", PB_D)])
                post_den()
            pending_den[0] = den_now

    g0 = proj_steps(0)
    for _ in g0:
        pass
    for hh in range(H):
        g = proj_steps(hh + 1) if hh + 1 < H else None
        attention(hh, g)
        if g is not None:
            for _ in g:
                pass
    if pending_den[0] is not None:
        pending_den[0]()
        pending_den[0] = None
    flush(all_=True)

    sc.add("act", lambda h: h.activation(out=stat_sb[:, 0:NT], in_=ps[PB_STAT][:, 0:NT], func=AF.Ln, scale=1.0 / (CC * P), bias=EPS),
           reads=[("ps", PB_STAT)], writes=[("stat", 0)])
    sc.add("act", lambda h: h.activation(out=stat_sb[:, NT:2 * NT], in_=ps[PB_STAT][:, NT:2 * NT], func=AF.Ln, scale=1.0 / (H * P), bias=EPS),
           reads=[("ps", PB_STAT)], writes=[("stat", 1)])
    sc.add("act", lambda h: h.activation(out=stat_sb[:, :], in_=stat_sb[:, :], func=AF.Exp, scale=-0.5),
           reads=[("stat", 0), ("stat", 1)], writes=[("stat", 0), ("stat", 1)])

    HK = [k_hT(k, c) for k in range(KC) for c in range(NPC)]
    hk_deps = set()
    for k_ in HK:
        hk_deps |= set(sc.readers.get(k_, ()))
    first_use = set()

    def hk(key):
        if key in first_use:
            return ()
        first_use.add(key)
        return list(hk_deps)
    banks3 = rr([0, 1, 2, 3, 4, 5])
    x_t = x_d.rearrange("(t p) d -> t p d", p=P)
    o_t = out_d.rearrange("(t p) d -> t p d", p=P)
    outs = []
    iters = [(n, t) for n in range(ND) for t in range(NT)]
    PF = XR - 1

    def issue_x(i):
        n, t = iters[i]
        ld("sp", xres[:, i % XR, :], x_t[t, :, n * 512:(n + 1) * 512], [("xres", i % XR)], extra=hk(("xres", i % XR)))

    for i in range(min(PF, len(iters))):
        issue_x(i)
    for it, (n, t) in enumerate(iters):
        wb = n % 2
        if t == 0:
            ld("pool", wo[wb][:, :, :].rearrange("p e c -> p (e c)"), wout_d[n, :, :], [("wo", wb)], extra=hk(("wo", wb)))
        if it + PF < len(iters):
            issue_x(it + PF)
        rx = it % XR
        r3 = it % 3
        r2 = it % 2
        b1 = banks3()
        b2 = banks3()

        def f(h, t=t, wb=wb, b1=b1, b2=b2):
            ins = None
            for e in range(KC):
                bank = b1 if e < CC else b2
                ee = e if e < CC else e - CC
                last = (CC - 1) if e < CC else (KC - CC - 1)
                ins = h.matmul(ps[bank][:, :], lhsT=yT[:, e, t * P:(t + 1) * P], rhs=wo[wb][:, e, :], start=(ee == 0), stop=(ee == last))
            return ins
        sc.add("pe", f, reads=[("wo", wb)] + [k_yT(e, t // 4) for e in range(KC)], writes=[("ps", b1), ("ps", b2)])
        sc.add("dve", lambda h, t=t, b1=b1, rx=rx, r2=r2: h.scalar_tensor_tensor(
            out=t1[:, r2, :], in0=ps[b1][:, :], scalar=stat_sb[:, t:t + 1], in1=xres[:, rx, :], op0=ALU.mult, op1=ALU.add),
            reads=[("ps", b1), ("stat", 0), ("xres", rx)], writes=[("t1", r2)], extra=hk(("t1", r2)))
        sc.add("dve", lambda h, t=t, b2=b2, r3=r3, r2=r2: h.scalar_tensor_tensor(
            out=ot[:, r3, :], in0=ps[b2][:, :], scalar=stat_sb[:, NT + t:NT + t + 1], in1=t1[:, r2, :], op0=ALU.mult, op1=ALU.add),
            reads=[("ps", b2), ("stat", 1), ("t1", r2)], writes=[("ot", r3)], extra=hk(("ot", r3)))
        outs.append(sc.add("act", lambda h, t=t, n=n, r3=r3: h.dma_start(out=o_t[t, :, n * 512:(n + 1) * 512], in_=ot[:, r3, :]),
                           reads=[("ot", r3)], writes=[], dma=True))

    for name in dbg:
        src = {"hT": hT, "yT": yT, "qT": qT, "kT": kT, "Vt": Vt, "zs": zs, "negmT": negmT, "stat": stat_sb}[name]
        shp = list(src.shape)
        dt_ = BF16 if src.dtype == BF16 else F32
        d = nc.dram_tensor("dbg_" + name, shp, dt_, kind="ExternalOutput").ap()
        keys = {"hT": HK, "yT": [k_yT(e, c) for e in range(KC) for c in range(NPC)], "qT": [("qT", hp_, c) for hp_ in range(2) for c in range(NPC)],
                "kT": [("kT", hp_, c) for hp_ in range(2) for c in range(NPC)], "Vt": [("Vt", hp_, c) for hp_ in range(2) for c in range(NPC)], "zs": [("zs", hp_, c) for hp_ in range(2) for c in range(NPC)],
                "negmT": [("negmT", 0), ("negmT", 1)], "stat": [("stat", 0), ("stat", 1)]}[name]
        full = tuple(slice(None) for _ in shp)
        outs.append(sc.add("sp", lambda h, d=d, src=src, full=full: h.dma_start(out=d[full], in_=src[full]), reads=keys, dma=True))

    sc.add("sp", None, extra=outs, name="final")

    return _emit(nc, sc)


def _emit(nc, sc):
    import contextlib
    with contextlib.ExitStack() as es:
        csem = {e: es.enter_context(nc.semaphore("c_" + e)) for e in ("pe", "act", "dve", "pool")}
        dsems = {e: [es.enter_context(nc.semaphore("d_%s%d" % (e, i))) for i in range(NDMA)] for e in ("sp", "pool", "act")}
        dsems["dve"] = dsems["pe"] = []
        sc.finalize(csem, dsems)
        block = es.enter_context(nc.Block())

        @block.tensor
        def _(h):
            sc.emit("pe", h)

        @block.scalar
        def _(h):
            sc.emit("act", h)

        @block.vector
        def _(h):
            sc.emit("dve", h)

        @block.gpsimd
        def _(h):
            sc.emit("pool", h)

        @block.sync
        def _(h):
            sc.emit("sp", h)
    return nc


def make_kst():
    k = np.zeros((P, NKST), np.float32)
    k[:, K_ID:K_ID + P] = np.eye(P, dtype=np.float32)
    kk = np.arange(P)[:, None]
    qq = np.arange(P)[None, :]
    k[:, K_TRI:K_TRI + P] = np.where(kk <= qq, 0.0, -BIG)
    for j in range(NBLK):
        k[j, K_SEL + j * P:K_SEL + (j + 1) * P] = 1.0
    cand = np.zeros((8, 8), np.float32)
    fix = np.zeros((8, 8), np.float32)
    for tq in range(8):
        own = (8 + tq) // 2
        for j in range(8):
            cand[tq, j] = 0.0 if j < own else -1e30
            fix[tq, j] = 0.0 if j == own else -BIG
    k[:, K_CAND:K_CAND + 64] = cand.reshape(1, 64)
    k[:, K_FIX:K_FIX + 64] = fix.reshape(1, 64)
    return k


def host_layout(x, norm_gain, w_in, conv_w, q_norm_gain, k_norm_gain, conv_out_gain, attn_out_gain, w_out):
    B, S_, D = x.shape
    KC = D // P
    CC = KC // 2
    H = KC // 2
    NCH = 4 * KC
    ND = D // 512
    f = np.float32
    w_in_r = np.ascontiguousarray(np.asarray(w_in[0], f).reshape(KC, P, NCH, P).transpose(2, 1, 0, 3)).reshape(NCH, P, KC * P)
    w_out_r = np.ascontiguousarray(np.asarray(w_out[0], f).reshape(KC, P, ND, 512).transpose(2, 1, 0, 3)).reshape(ND, P, KC * 512)
    cst = np.concatenate([
        np.asarray(norm_gain[0], f).reshape(KC, P).T,
        np.asarray(conv_w[0], f).reshape(3, CC, P).transpose(2, 1, 0).reshape(P, 3 * CC),
        np.asarray(conv_out_gain[0], f).reshape(CC, P).T,
        np.asarray(attn_out_gain[0], f).reshape(H, P).T,
        np.asarray(q_norm_gain[0], f).reshape(P, 1),
        np.asarray(k_norm_gain[0], f).reshape(P, 1),
    ], axis=1)
    cst = np.ascontiguousarray(cst, dtype=f)
    kst = make_kst()
    maps = []
    for b in range(B):
        xb = np.ascontiguousarray(np.asarray(x[b], f))
        maps.append({"xT": np.ascontiguousarray(xb.T), "x": xb, "w_in": w_in_r, "w_out": w_out_r, "cst": cst, "kst": kst})
    return maps


_NC_CACHE = {}


def kernel(x, norm_gain, w_in, conv_w, q_norm_gain, k_norm_gain, conv_out_gain, attn_out_gain, w_out):
    x = np.asarray(x)
    B, S_, D = x.shape
    assert S_ == S
    maps = host_layout(x, np.asarray(norm_gain), np.asarray(w_in), np.asarray(conv_w), np.asarray(q_norm_gain),
                       np.asarray(k_norm_gain), np.asarray(conv_out_gain), np.asarray(attn_out_gain), np.asarray(w_out))
    nc = build(D)
    res = run_bass_kernel_spmd(nc, maps, core_ids=list(range(B)))
    return np.stack([np.asarray(r["out"], np.float32) for r in res.results], axis=0)
```

```python
import numpy as np
import concourse.bass as bass
import concourse.mybir as mybir
from concourse.bass_utils import run_bass_kernel_spmd

F32 = mybir.dt.float32
BF16 = mybir.dt.bfloat16
AF = mybir.ActivationFunctionType
ALU = mybir.AluOpType
AX = mybir.AxisListType

P = 128
S = 2048
BLK = 256
NBLK = S // BLK
TOPK = 3
BIG = 32768.0
EPS = 1e-6
NPC = S // 512
NT = S // P
NSLOT = 4
NDMA = 8

K_ID, K_TRI, K_SEL, K_CAND, K_FIX = 0, 128, 256, 256 + 1024, 256 + 1024 + 64
NKST = K_FIX + 64


class Task:
    __slots__ = ("eng", "fn", "deps", "pos", "sig", "sigval", "dma", "dsem", "dval", "dprev", "name")

    def __init__(self, eng, fn, dma, name):
        self.eng = eng
        self.fn = fn
        self.dma = dma
        self.name = name
        self.deps = set()
        self.pos = -1
        self.sig = False
        self.sigval = 0
        self.dsem = None
        self.dval = 0
        self.dprev = None


class Sched:
    ENGS = ("pe", "act", "dve", "pool", "sp")

    def __init__(self):
        self.tasks = {e: [] for e in self.ENGS}
        self.last_w = {}
        self.readers = {}
        self.ndma = {e: 0 for e in self.ENGS}

    def add(self, eng, fn, reads=(), writes=(), dma=False, name="", extra=()):
        t = Task(eng, fn, dma, name)
        deps = set(extra)
        for k in reads:
            w = self.last_w.get(k)
            if w is not None:
                deps.add(w)
        for k in writes:
            w = self.last_w.get(k)
            if w is not None:
                deps.add(w)
            for r in self.readers.get(k, ()):
                deps.add(r)
        for k in reads:
            self.readers.setdefault(k, []).append(t)
        for k in writes:
            self.last_w[k] = t
            self.readers[k] = []
        deps.discard(t)
        t.pos = len(self.tasks[eng])
        self.tasks[eng].append(t)
        best = {}
        keep = []
        for d in deps:
            if d.dma:
                keep.append(d)
                continue
            if d.eng == "pe" and eng == "pe" and not dma:
                continue
            b = best.get(d.eng)
            if b is None or d.pos > b.pos:
                best[d.eng] = d
        keep.extend(best.values())
        t.deps = keep
        for d in keep:
            d.sig = True
        if dma:
            t.sig = True
        return t

    def last(self, eng):
        ts = self.tasks[eng]
        return ts[-1] if ts else None

    def barrier(self, engs):
        lasts = [self.last(e) for e in self.ENGS]
        lasts = [t for t in lasts if t is not None]
        for e in engs:
            self.add(e, None, extra=[t for t in lasts if t.eng != e], name="barrier")

    def finalize(self, csem, dsems):
        for e in self.ENGS:
            cnt = 0
            nd = 0
            for t in self.tasks[e]:
                if t.dma:
                    t.dsem = dsems[e][nd % NDMA]
                    t.dval = 16 * (nd // NDMA + 1)
                    nd += 1
                elif t.sig and t.fn is not None:
                    cnt += 1
                    t.sigval = cnt
                elif t.sig:
                    raise RuntimeError("no-op task used as dependency")
        self.csem = csem

    def emit(self, e, h):
        waited = {}

        def wait(sem, key, val):
            if waited.get(key, 0) < val:
                h.wait_ge(sem, val)
                waited[key] = val

        nd = 0
        for t in self.tasks[e]:
            for d in t.deps:
                if d.dma:
                    wait(d.dsem, ("d", d.eng, id(d.dsem)), d.dval)
                else:
                    wait(self.csem[d.eng], ("c", d.eng), d.sigval)
            if t.dma:
                if t.dval > 16:
                    wait(t.dsem, ("d", e, id(t.dsem)), t.dval - 16)
                ins = t.fn(h)
                ins.then_inc(t.dsem, 16)
                nd += 1
            elif t.fn is not None:
                ins = t.fn(h)
                if t.sig:
                    ins.then_inc(self.csem[e], 1)


def build(D, dbg=()):
    KC = D // P
    CC = KC // 2
    H = KC // 2
    NCH = 4 * KC
    ND = D // 512
    NCST = KC + 3 * CC + CC + H + 2
    C_GX, C_CW, C_GC, C_GA, C_GQ, C_GK = 0, KC, KC + 3 * CC, KC + 4 * CC, KC + 4 * CC + H, KC + 4 * CC + H + 1

    nc = bass.Bass("TRN2", target_bir_lowering=False)
    xT_d = nc.dram_tensor("xT", [D, S], F32, kind="ExternalInput").ap()
    x_d = nc.dram_tensor("x", [S, D], F32, kind="ExternalInput").ap()
    win_d = nc.dram_tensor("w_in", [NCH, P, KC * P], F32, kind="ExternalInput").ap()
    wout_d = nc.dram_tensor("w_out", [ND, P, KC * 512], F32, kind="ExternalInput").ap()
    cst_d = nc.dram_tensor("cst", [P, NCST], F32, kind="ExternalInput").ap()
    kst_d = nc.dram_tensor("kst", [P, NKST], F32, kind="ExternalInput").ap()
    out_d = nc.dram_tensor("out", [S, D], F32, kind="ExternalOutput").ap()
    dbg_d = {}

    SB_BASE, SB_END = 16512, 229344
    off = [SB_BASE]

    def alloc(name, shape, dt, at=None):
        nbytes = int(np.prod(shape[1:])) * (4 if dt == F32 else 2)
        if at is None:
            at = off[0]
            off[0] = (at + nbytes + 31) // 32 * 32
        return nc.alloc_sbuf_tensor_at(name, list(shape), dt, offset=at)

    HT_OFF = SB_BASE
    hT = alloc("hT", [P, KC, S], BF16)
    YT_OFF = off[0]
    yT = alloc("yT", [P, KC, S], BF16)
    wring = alloc("wring", [P, NSLOT, KC * P], BF16)
    cst = alloc("cst_sb", [P, NCST], F32)
    ident_bf = alloc("ident_bf", [P, P], BF16)
    tri_bf = alloc("tri_bf", [P, P], BF16)
    ones_bf = alloc("ones_bf", [P, P], BF16)
    selj_bf = alloc("selj_bf", [P, NBLK * P], BF16)
    ident_f = alloc("ident_f", [P, P], F32)
    cand_f = alloc("cand_f", [P, 64], F32)
    fixc_f = alloc("fixc_f", [P, 64], F32)
    nbias = alloc("nbias", [P, 1], F32)
    gqs = alloc("gqs", [P, 2], F32)
    stat_sb = alloc("stat_sb", [P, 2 * NT], F32)
    kms = alloc("kms", [P, NBLK], F32)
    kmean_bf = alloc("kmean_bf", [P, NBLK], BF16)
    ones_f = alloc("ones_f", [P, P], F32)
    SCR = off[0]
    off[0] = SCR
    hs = alloc("hs", [P, NPC, 512], F32)
    u = alloc("u", [P, 2 + S + 6], F32)
    a_t = alloc("a_t", [P, NPC, 512], F32)
    y_t = alloc("y_t", [P, 2, 512], F32)
    sqc = alloc("sqc", [P, 2, 512], BF16)
    sz1 = alloc("sz1", [P, NPC, 512], F32)
    sq0 = alloc("sq0", [P, 2, 4, 512], BF16)
    rstd = alloc("rstd", [P, 2, 512], F32)
    prm = alloc("prm", [P, P], F32)
    onesf = alloc("onesf", [P, P], F32)
    END1 = off[0]
    off[0] = SCR
    qf = alloc("qf", [P, 2, 512], F32)
    sqq = alloc("sqq", [P, 2, 512], BF16)
    rq = alloc("rq", [P, 1, 512], F32)
    qT = alloc("qT", [P, 2, S], BF16)
    kT = alloc("kT", [P, 2, S], BF16)
    Vt = alloc("Vt", [P, 2, NT, P], BF16)
    zs = alloc("zs", [P, 2, S], BF16)
    pT = alloc("pT", [P, 3, 512], BF16)
    rden = alloc("rden", [P, 1, 512], F32)
    acc = alloc("acc", [P, 512], F32)
    at_t = alloc("at_t", [P, 1, 512], F32)
    sqa = alloc("sqa", [P, 2, 512], BF16)
    gm = alloc("gm", [P, 64], F32)
    top8 = alloc("top8", [P, 8, 8], F32)
    negm_pad = alloc("negm_pad", [P, 8, P], BF16)
    negmT = alloc("negmT", [P, 2, 8 * P], BF16)
    END2 = off[0]
    assert max(END1, END2) <= SB_END, (END1, END2)
    WO_BYTES = KC * 512 * 2
    wo = [alloc("wo%d" % i, [P, KC, 512], BF16, at=HT_OFF + i * WO_BYTES) for i in range(2)]
    p3 = HT_OFF + 2 * WO_BYTES
    XR = 4 if KC >= 16 else 2
    xres = alloc("xres", [P, XR, 512], F32, at=p3)
    t1 = alloc("t1", [P, 2, 512], F32, at=p3 + XR * 2048)
    ot = alloc("ot", [P, 3, 512], F32, at=p3 + XR * 2048 + 4096)
    assert p3 + XR * 2048 + 4096 + 6144 <= HT_OFF + KC * S * 2
    xs = alloc("xs", [P, 2, KC, 512], F32, at=YT_OFF)
    assert 2 * KC * 512 * 4 <= KC * S * 2

    ps = [nc.alloc_psum_tensor("ps%d" % i, [P, 512], F32) for i in range(8)]
    ps2_bf = ps[2].bitcast(BF16)
    PB_MISC, PB_STAT = 2, 7

    sc = Sched()

    def k_hT(k, c):
        return ("hT", k, c)

    def k_yT(e, c):
        return ("yT", e, c)

    def ld(eng, out_ap, in_ap, writes, reads=(), extra=()):
        return sc.add(eng, lambda h, o=out_ap, i=in_ap: h.dma_start(out=o, in_=i), reads=reads, writes=writes, dma=True, extra=extra)

    ld("sp", cst[:, :], cst_d[:, :], [("cst",)])
    ld("sp", ident_f[:, :], kst_d[:, K_ID:K_ID + P], [("ident_f",)])
    ld("sp", cand_f[:, :], kst_d[:, K_CAND:K_CAND + 64], [("cand",)])
    ld("sp", fixc_f[:, :], kst_d[:, K_FIX:K_FIX + 64], [("fixc",)])
    ld("pool", ident_bf[:, :], kst_d[:, K_ID:K_ID + P], [("ident_bf",)])
    ld("pool", tri_bf[:, :], kst_d[:, K_TRI:K_TRI + P], [("tri",)])
    ld("pool", selj_bf[:, :], kst_d[:, K_SEL:K_SEL + NBLK * P], [("selj",)])
    sc.add("dve", lambda h: h.memset(ones_bf[:, :], 1.0), writes=[("ones",)])
    sc.add("dve", lambda h: h.memset(onesf[:, :], 1.0), writes=[("onesf",)])
    sc.add("dve", lambda h: h.memset(ones_f[:, :], 1.0), writes=[("ones_f",)])
    sc.add("dve", lambda h: h.memset(u[:, 0:2], 0.0), writes=[("u", -1)])

    seq = []
    for cc in range(CC):
        seq += [cc, 2 * CC + cc, 3 * CC + cc, CC + cc]
    for hh in range(H):
        seq += [4 * CC + 3 * H + hh, 4 * CC + hh, 4 * CC + H + hh, 4 * CC + 2 * H + hh]
    wnext = [0]

    def issue_w(extra=()):
        i = wnext[0]
        if i >= len(seq):
            return
        wnext[0] += 1
        slot = i % NSLOT
        ld("pool", wring[:, slot, :], win_d[seq[i], :, :], [("w", slot)], extra=extra)

    issue_w()

    sc.add("dve", lambda h: h.tensor_tensor(out=gqs[:, 0:1], in0=cst[:, C_GQ:C_GQ + 1], in1=cst[:, C_GK:C_GK + 1], op=ALU.mult),
           reads=[("cst",)], writes=[("gqs",)])
    sc.add("dve", lambda h: h.tensor_scalar(out=prm[:, :], in0=ident_f[:, :], scalar1=gqs[:, 0:1], scalar2=None, op0=ALU.mult),
           reads=[("gqs",), ("ident_f",)], writes=[("prm",)])
    sc.add("pe", lambda h: h.matmul(ps[5][:, 0:P], lhsT=onesf[:, :], rhs=prm[:, :], start=True, stop=True),
           reads=[("prm",), ("onesf",)], writes=[("ps", 5)])
    sc.add("dve", lambda h: h.tensor_reduce(out=gqs[:, 1:2], in_=ps[5][:, 0:P], axis=AX.X, op=ALU.max, apply_absolute_value=True),
           reads=[("ps", 5)], writes=[("gqs2",)])
    sc.add("dve", lambda h: h.tensor_scalar(out=nbias[:, :], in0=gqs[:, 1:2], scalar1=-float(np.sqrt(P)), scalar2=None, op0=ALU.mult),
           reads=[("gqs2",)], writes=[("nbias",)])

    xT_r = xT_d.rearrange("(k p) t -> p k t", p=P)
    KG = KC // 4
    def prologue_chunk(c):
        half = c % 2
        xl = []
        for g in range(KG):
            xl.append(ld("sp", xs[:, half, 4 * g:4 * g + 4, :], xT_r[:, 4 * g:4 * g + 4, c * 512:(c + 1) * 512],
                         [("xs", half, g)]))
        if c == 0:
            for _ in range(NSLOT - 1):
                issue_w(extra=xl)
        for g in range(KG):
            b = (c * KG + g) % 2
            sc.add("act", lambda h, b=b, half=half, g=g: h.activation(out=sq0[:, b, :, :], in_=xs[:, half, 4 * g:4 * g + 4, :], func=AF.Square),
                   reads=[("xs", half, g)], writes=[("sq0", b)])

            def f(h, b=b, g=g):
                ins = None
                for kk in range(4):
                    ins = h.matmul(ps[6][:, :], lhsT=ones_bf[:, :], rhs=sq0[:, b, kk, :],
                                   start=(g == 0 and kk == 0), stop=(g == KG - 1 and kk == 3))
                return ins
            sc.add("pe", f, reads=[("sq0", b), ("ones",)], writes=[("ps", 6)])
        r = c % 2
        sc.add("act", lambda h, r=r: h.activation(out=rstd[:, r, :], in_=ps[6][:, :], func=AF.Ln, scale=1.0 / D, bias=EPS),
               reads=[("ps", 6)], writes=[("rstd", r)])
        sc.add("act", lambda h, r=r: h.activation(out=rstd[:, r, :], in_=rstd[:, r, :], func=AF.Exp, scale=-0.5),
               reads=[("rstd", r)], writes=[("rstd", r)])
        for k in range(KC):
            sc.add("dve", lambda h, k=k, c=c, half=half, r=r: h.scalar_tensor_tensor(
                out=hT[:, k, c * 512:(c + 1) * 512], in0=xs[:, half, k, :], scalar=cst[:, C_GX + k:C_GX + k + 1],
                in1=rstd[:, r, :], op0=ALU.mult, op1=ALU.mult),
                reads=[("xs", half, k // 4), ("rstd", r), ("cst",)], writes=[k_hT(k, c)])

    def xs_readers():
        d = set()
        for half_ in range(2):
            for g_ in range(KG):
                d |= set(sc.readers.get(("xs", half_, g_), ()))
                w_ = sc.last_w.get(("xs", half_, g_))
                if w_ is not None:
                    d.add(w_)
        return list(d)

    pend = []

    def defer(fn, reads, writes, age=1, then=None):
        pend.append([age, fn, reads, writes, then])

    def flush(all_=False):
        keep = []
        todo = list(pend)
        pend[:] = []
        for it in todo:
            if it[0] <= 0 or all_:
                sc.add("pe", it[1], reads=it[2], writes=it[3])
                if it[4] is not None:
                    it[4]()
            else:
                it[0] -= 1
                keep.append(it)
        pend[:] = keep + pend

    widx = [0]

    def slab_gen(banks, on_group, v_mode=False):
        i = widx[0]
        widx[0] += 1
        slot = i % NSLOT
        KH = KC // 2
        for c in range(NPC):
            bank = banks()
            for half in range(2):
                if not v_mode:
                    def f(h, c=c, bank=bank, slot=slot, half=half):
                        ins = None
                        for k in range(half * KH, (half + 1) * KH):
                            ins = h.matmul(ps[bank][:, :], lhsT=wring[:, slot, k * P:(k + 1) * P], rhs=hT[:, k, c * 512:(c + 1) * 512],
                                           start=(k == 0), stop=(k == KC - 1))
                        return ins
                else:
                    def f(h, c=c, bank=bank, slot=slot, half=half):
                        ins = None
                        for tt in range(2 * half, 2 * half + 2):
                            t = 4 * c + tt
                            for k in range(KC):
                                ins = h.matmul(ps[bank][:, tt * P:(tt + 1) * P], lhsT=hT[:, k, t * P:(t + 1) * P],
                                               rhs=wring[:, slot, k * P:(k + 1) * P], start=(k == 0), stop=(k == KC - 1))
                        return ins
                sc.add("pe", f, reads=[("w", slot)] + [k_hT(k, c) for k in range(KC)], writes=[("ps", bank)])
                if half == 0:
                    yield
            flush()
            on_group(c, bank)
            if c == NPC - 1:
                issue_w()
            yield

    def slab(banks, on_group, v_mode=False):
        for _ in slab_gen(banks, on_group, v_mode):
            pass

    def rr(banks_list):
        st = [0]

        def nxt():
            b = banks_list[st[0] % len(banks_list)]
            st[0] += 1
            return b
        return nxt

    stat_started = [False]

    def stats_mm(src, col0):
        def f(h, src=src, col0=col0):
            ins = None
            for tt in range(4):
                first = not stat_started[0]
                stat_started[0] = True
                ins = h.matmul(ps[PB_STAT][:, col0 + tt:col0 + tt + 1], lhsT=src[:, tt * P:(tt + 1) * P], rhs=ones_bf[:, 0:1],
                               start=first, stop=True, skip_group_check=True)
            return ins
        return f

    banks1 = rr([0, 1, 2, 3, 4, 5])
    for cc in range(CC):
        def on_h(c, bank):
            sc.add("act", lambda h, c=c, bank=bank: h.activation(out=hs[:, c, :], in_=ps[bank][:, :], func=AF.Copy),
                   reads=[("ps", bank)], writes=[("hs", c)])

        def on_c(c, bank, cc=cc):
            sc.add("dve", lambda h, c=c, bank=bank: h.tensor_tensor(out=u[:, 2 + c * 512:2 + (c + 1) * 512], in0=ps[bank][:, :],
                                                                    in1=hs[:, c, :], op=ALU.mult),
                   reads=[("ps", bank), ("hs", c)], writes=[("u", c)])
            r = c
            w0 = cst[:, C_CW + 3 * cc + 0:C_CW + 3 * cc + 1]
            w1 = cst[:, C_CW + 3 * cc + 1:C_CW + 3 * cc + 2]
            w2 = cst[:, C_CW + 3 * cc + 2:C_CW + 3 * cc + 3]
            sc.add("act", lambda h, c=c, r=r, w2=w2: h.activation(out=a_t[:, r, :], in_=u[:, 2 + c * 512:2 + (c + 1) * 512],
                                                                   func=AF.Identity, scale=w2),
                   reads=[("u", c), ("cst",)], writes=[("a", r)])
            sc.add("dve", lambda h, c=c, r=r, w1=w1: h.scalar_tensor_tensor(out=a_t[:, r, :], in0=u[:, 1 + c * 512:1 + (c + 1) * 512],
                                                                            scalar=w1, in1=a_t[:, r, :], op0=ALU.mult, op1=ALU.add),
                   reads=[("u", c), ("u", c - 1), ("a", r)], writes=[("a", r)])
            sc.add("dve", lambda h, c=c, r=r, w0=w0: h.scalar_tensor_tensor(out=a_t[:, r, :], in0=u[:, c * 512:(c + 1) * 512],
                                                                            scalar=w0, in1=a_t[:, r, :], op0=ALU.mult, op1=ALU.add),
                   reads=[("u", c), ("u", c - 1), ("a", r)], writes=[("a", r)])

        def on_z(c, bank):
            sc.add("act", lambda h, c=c, bank=bank: h.activation(out=sz1[:, c, :], in_=ps[bank][:, :], func=AF.Silu),
                   reads=[("ps", bank)], writes=[("sz1", c)])

        def on_b(c, bank, cc=cc):
            r = c % 2
            sc.add("dve", lambda h, r=r, c=c, bank=bank: h.tensor_tensor(out=y_t[:, r, :], in0=ps[bank][:, :], in1=a_t[:, c, :], op=ALU.mult),
                   reads=[("ps", bank), ("a", c)], writes=[("y", r)])
            sc.add("act", lambda h, r=r: h.activation(out=sqc[:, r, :], in_=y_t[:, r, :], func=AF.Square),
                   reads=[("y", r)], writes=[("sqc", r)])
            defer(stats_mm(sqc[:, r, :], 4 * c), [("sqc", r), ("ones",)], [("ps", PB_STAT)], age=1)
            gcc = cst[:, C_GC + cc:C_GC + cc + 1]
            sc.add("dve", lambda h, r=r, c=c, cc=cc, gcc=gcc: h.scalar_tensor_tensor(
                out=yT[:, cc, c * 512:(c + 1) * 512], in0=y_t[:, r, :], scalar=gcc, in1=sz1[:, c, :], op0=ALU.mult, op1=ALU.mult),
                reads=[("y", r), ("sz1", c), ("cst",)], writes=[k_yT(cc, c)], extra=xs_readers())

        if cc == 0:
            g_h = slab_gen(banks1, on_h)
            g_c = slab_gen(banks1, on_c)
            prologue_chunk(0)
            for c in range(NPC):
                if c + 1 < NPC:
                    prologue_chunk(c + 1)
                next(g_h, None)
                next(g_h, None)
                next(g_c, None)
                next(g_c, None)
            for _ in g_h:
                pass
            for _ in g_c:
                pass
        else:
            slab(banks1, on_h)
            slab(banks1, on_c)
        slab(banks1, on_z)
        slab(banks1, on_b)
    flush(all_=True)

    if "p1" in dbg:
        outs = []
        for name, src in (("hs", hs), ("u", u), ("a_t", a_t), ("y_t", y_t), ("sz1", sz1), ("hT", hT), ("yT", yT), ("cst", cst)):
            shp = list(src.shape)
            d = nc.dram_tensor("dbg_" + name, shp, src.dtype, kind="ExternalOutput").ap()
            full = tuple(slice(None) for _ in shp)
            sc.barrier(["sp"])
            outs.append(sc.add("sp", lambda h, d=d, src=src, full=full: h.dma_start(out=d[full], in_=src[full]), dma=True))
        sc.add("sp", None, extra=outs, name="final")
        return _emit(nc, sc)
    sc.barrier(["act", "dve"])
    sc.add("dve", lambda h: h.memset(negm_pad[:, :, :], 0.0), writes=[("negm_pad",)])

    banks2 = rr([0, 1])
    sbanks = rr([3, 4])
    PB_O, PB_D = 5, 6
    prr = [0]
    scale = float(P) ** -0.5

    def proj_steps(hh):
        hp = hh % 2
        def on_za(c, bank):
            sc.add("act", lambda h, c=c, bank=bank: h.activation(out=zs[:, hp, c * 512:(c + 1) * 512], in_=ps[bank][:, :], func=AF.Copy),
                   reads=[("ps", bank)], writes=[("zs", hp, c)])
        yield from slab_gen(banks2, on_za)
        sc.add("act", lambda h: h.activation(out=zs[:, hp, :], in_=zs[:, hp, :], func=AF.Silu),
               reads=[("zs", hp, c) for c in range(NPC)], writes=[("zs", hp, c) for c in range(NPC)])

        for which, dst, gcol in ((0, qT, C_GQ), (1, kT, C_GK)):
            key = "qT" if which == 0 else "kT"

            def on(c, bank, dst=dst, gcol=gcol, key=key):
                r = c % 2
                sc.add("act", lambda h, r=r, bank=bank: h.activation(out=qf[:, r, :], in_=ps[bank][:, :], func=AF.Copy),
                       reads=[("ps", bank)], writes=[("qf", r)])
                sc.add("act", lambda h, r=r, bank=bank: h.activation(out=sqq[:, r, :], in_=ps[bank][:, :], func=AF.Square),
                       reads=[("ps", bank)], writes=[("sqq", r)])

                def post(r=r, c=c, dst=dst, gcol=gcol, key=key):
                    sc.add("act", lambda h, r=r: h.activation(out=rq[:, 0, :], in_=ps[PB_MISC][:, :], func=AF.Ln, scale=1.0 / P, bias=EPS),
                           reads=[("ps", PB_MISC)], writes=[("rq",)])
                    sc.add("act", lambda h, r=r: h.activation(out=rq[:, 0, :], in_=rq[:, 0, :], func=AF.Exp, scale=-0.5),
                           reads=[("rq",)], writes=[("rq",)])
                    sc.add("dve", lambda h, r=r, c=c, dst=dst, gcol=gcol: h.scalar_tensor_tensor(
                        out=dst[:, hp, c * 512:(c + 1) * 512], in0=qf[:, r, :], scalar=cst[:, gcol:gcol + 1], in1=rq[:, 0, :],
                        op0=ALU.mult, op1=ALU.mult),
                        reads=[("qf", r), ("rq",), ("cst",)], writes=[(key, hp, c)])
                defer(lambda h, r=r: h.matmul(ps[PB_MISC][:, :], lhsT=ones_bf[:, :], rhs=sqq[:, r, :], start=True, stop=True),
                      [("sqq", r), ("ones",)], [("ps", PB_MISC)], age=1, then=post)
            yield from slab_gen(banks2, on)

        flush(all_=True)

        sc.add("dve", lambda h: h.tensor_reduce(out=kms[:, :], in_=kT[:, hp, :].rearrange("p (j w) -> p j w", w=BLK), axis=AX.X, op=ALU.add),
               reads=[("kT", hp, c) for c in range(NPC)], writes=[("kms",)])
        sc.add("dve", lambda h: h.tensor_scalar(out=kmean_bf[:, :], in0=kms[:, :], scalar1=1.0 / BLK, scalar2=None, op0=ALU.mult),
               reads=[("kms",)], writes=[("kmean",)])

        def f_gate(h):
            ins = None
            for tq in range(8):
                t = 8 + tq
                ins = h.matmul(ps[PB_MISC][:, tq * 8:(tq + 1) * 8], lhsT=qT[:, hp, t * P:(t + 1) * P], rhs=kmean_bf[:, :],
                               start=True, stop=True)
            return ins

        def post_gate():
            sc.add("dve", lambda h: h.tensor_tensor(out=gm[:, :], in0=ps[PB_MISC][:, 0:64], in1=cand_f[:, :], op=ALU.add),
                   reads=[("ps", PB_MISC), ("cand",)], writes=[("gm",)])
            for tq in range(8):
                sc.add("dve", lambda h, tq=tq: h.max(out=top8[:, tq, :], in_=gm[:, tq * 8:(tq + 1) * 8]),
                       reads=[("gm",)], writes=[("top8", tq)])
                sc.add("dve", lambda h, tq=tq: h.tensor_scalar(out=gm[:, tq * 8:(tq + 1) * 8], in0=gm[:, tq * 8:(tq + 1) * 8],
                                                               scalar1=top8[:, tq, TOPK - 1:TOPK], scalar2=BIG, op0=ALU.is_ge, op1=ALU.mult),
                       reads=[("gm",), ("top8", tq)], writes=[("gm",)])
            sc.add("dve", lambda h: h.tensor_tensor(out=negm_pad[:, :, 0:8], in0=gm[:, :].rearrange("p (t j) -> p t j", j=8),
                                                    in1=fixc_f[:, :].rearrange("p (t j) -> p t j", j=8), op=ALU.add),
                   reads=[("gm",), ("fixc",)], writes=[("negm_pad",)])
        defer(f_gate, [("qT", hp, 2), ("qT", hp, 3), ("kmean",)], [("ps", PB_MISC)], age=1, then=post_gate)

        def on_v(c, bank):
            sc.add("dve", lambda h, c=c, bank=bank: h.tensor_copy(out=Vt[:, hp, 4 * c:4 * c + 4, :],
                                                                  in_=ps[bank][:, :].rearrange("p (t d) -> p t d", d=P)),
                   reads=[("ps", bank)], writes=[("Vt", hp, c)])
        yield from slab_gen(banks2, on_v, v_mode=True)

        def f_tr(h):
            ins = None
            for tq in range(8):
                ins = h.transpose(out=ps2_bf[:, tq * P:(tq + 1) * P], in_=negm_pad[:, tq, :], identity=ident_bf[:, :])
            return ins
        flush(all_=True)
        sc.add("pe", f_tr, reads=[("negm_pad",), ("ident_bf",)], writes=[("ps", PB_MISC)])
        sc.add("dve", lambda h: h.tensor_copy(out=negmT[:, hp, :], in_=ps2_bf[:, :]), reads=[("ps", PB_MISC)], writes=[("negmT", hp)])
        yield

    pending_den = [None]

    def attention(hh, g):
        hp = hh % 2
        ntile = [0]

        def pull():
            if g is not None:
                next(g, None)

        for c in range(NPC):
            nk = 4 * c + 4
            tiles = []
            for kc in range(nk):
                i = kc - 4 * c
                qoff = 0 if i < 0 else P * i
                tiles.append((kc, qoff, 512 - qoff))

            def qk_parts(kc, qoff, N, c=c):
                sb = sbanks()
                j = kc // 2
                q0 = 512 * c + qoff
                mms = [("qk",)]
                for b in (2 * c, 2 * c + 1):
                    if b >= 4 and j < b:
                        lo = max(b * BLK, q0)
                        hi = (b + 1) * BLK
                        if hi > lo:
                            mms.append(("sel", lo - q0, hi - q0, lo - 4 * BLK))
                if kc >= 4 * c:
                    mms.append(("tri",))

                def f(h, kc=kc, q0=q0, N=N, sb=sb, mms=mms, j=j):
                    ins = None
                    for n_, m in enumerate(mms):
                        st, sp_ = (n_ == 0), (n_ == len(mms) - 1)
                        if m[0] == "qk":
                            ins = h.matmul(ps[sb][:, 0:N], lhsT=kT[:, hp, kc * P:(kc + 1) * P], rhs=qT[:, hp, q0:q0 + N], start=st, stop=sp_)
                        elif m[0] == "sel":
                            ins = h.matmul(ps[sb][:, m[1]:m[2]], lhsT=selj_bf[:, j * P:(j + 1) * P],
                                           rhs=negmT[:, hp, m[3]:m[3] + (m[2] - m[1])], start=st, stop=sp_)
                        else:
                            ins = h.matmul(ps[sb][:, 0:P], lhsT=ident_bf[:, :], rhs=tri_bf[:, :], start=st, stop=sp_)
                    return ins
                reads = [("kT", hp, kc // 4), ("qT", hp, c), ("negmT", hp), ("selj",), ("ident_bf",), ("tri",)]
                pr = prr[0] % 3
                prr[0] += 1
                return f, reads, sb, pr, N

            def add_exp(sb, pr, N):
                sc.add("act", lambda h, sb=sb, N=N, pr=pr: h.activation(out=pT[:, pr, 0:N], in_=ps[sb][:, 0:N], func=AF.Exp,
                                                                        scale=scale, bias=nbias[:, 0:1]),
                       reads=[("ps", sb), ("nbias",)], writes=[("pT", pr)])

            def add_pool(pr, N, first=False):
                if first:
                    sc.add("pool", lambda h, pr=pr: h.tensor_copy(out=acc[:, :], in_=pT[:, pr, :]),
                           reads=[("pT", pr)], writes=[("acc",)])
                else:
                    sc.add("pool", lambda h, pr=pr, N=N: h.tensor_tensor(out=acc[:, 512 - N:512], in0=acc[:, 512 - N:512], in1=pT[:, pr, 0:N], op=ALU.add),
                           reads=[("pT", pr), ("acc",)], writes=[("acc",)])

            def pv_fn(kc, qoff, N, pr, nk=nk):
                def f(h, kc=kc, qoff=qoff, N=N, pr=pr):
                    return h.matmul(ps[PB_O][:, qoff:512], lhsT=Vt[:, hp, kc, :], rhs=pT[:, pr, 0:N], start=(kc == 0), stop=(kc == nk - 1))
                return f, [("pT", pr), ("Vt", hp, kc // 4)]

            qk = [None] * nk
            for n_ in range(min(2, nk)):
                qk[n_] = qk_parts(*tiles[n_])
                sc.add("pe", qk[n_][0], reads=qk[n_][1], writes=[("ps", qk[n_][2])])
                add_exp(qk[n_][2], qk[n_][3], qk[n_][4])
            if pending_den[0] is not None:
                pending_den[0]()
                pending_den[0] = None
            for n_ in range(min(2, nk)):
                add_pool(qk[n_][3], qk[n_][4], first=(n_ == 0))
            for n_ in range(nk):
                if ntile[0] % 8 != 7:
                    pull()
                fpv, rpv = pv_fn(*tiles[n_], qk[n_][3])
                if n_ + 2 < nk:
                    qk[n_ + 2] = qk_parts(*tiles[n_ + 2])
                    fq, rq_, sbq = qk[n_ + 2][0], qk[n_ + 2][1], qk[n_ + 2][2]

                    def fboth(h, fpv=fpv, fq=fq):
                        fpv(h)
                        return fq(h)
                    sc.add("pe", fboth, reads=rpv + rq_, writes=[("ps", PB_O), ("ps", sbq)])
                    add_exp(sbq, qk[n_ + 2][3], qk[n_ + 2][4])
                    add_pool(qk[n_ + 2][3], qk[n_ + 2][4])
                else:
                    sc.add("pe", fpv, reads=rpv, writes=[("ps", PB_O)])
                flush()
                ntile[0] += 1
            sc.add("act", lambda h: h.activation(out=at_t[:, 0, :], in_=ps[PB_O][:, :], func=AF.Copy),
                   reads=[("ps", PB_O)], writes=[("at",)])
            r = c % 2
            gah = cst[:, C_GA + hh:C_GA + hh + 1]

            def post_den(c=c, r=r, gah=gah):
                sc.add("dve", lambda h: h.reciprocal(out=rden[:, 0, :], in_=ps[PB_D][:, :]),
                       reads=[("ps", PB_D)], writes=[("rden",)])
                sc.add("dve", lambda h: h.tensor_tensor(out=at_t[:, 0, :], in0=at_t[:, 0, :], in1=rden[:, 0, :], op=ALU.mult),
                       reads=[("at",), ("rden",)], writes=[("at",)])
                sc.add("act", lambda h, r=r: h.activation(out=sqa[:, r, :], in_=at_t[:, 0, :], func=AF.Square),
                       reads=[("at",)], writes=[("sqa", r)])
                defer(stats_mm(sqa[:, r, :], NT + 4 * c), [("sqa", r), ("ones",)], [("ps", PB_STAT)], age=7)
                sc.add("dve", lambda h, c=c, gah=gah: h.scalar_tensor_tensor(
                    out=yT[:, CC + hh, c * 512:(c + 1) * 512], in0=at_t[:, 0, :], scalar=gah, in1=zs[:, hp, c * 512:(c + 1) * 512],
                    op0=ALU.mult, op1=ALU.mult),
                    reads=[("at",), ("zs", hp, c), ("cst",)], writes=[k_yT(CC + hh, c)], extra=xs_readers())
            def den_now(post_den=post_den):
                sc.add("pe", lambda h: h.matmul(ps[PB_D][:, :], lhsT=ones_f[:, :], rhs=acc[:, :], start=True, stop=True),
                       reads=[("acc",), ("ones_f",)], writes=[("ps", PB_D)])
                post_den()
            pending_den[0] = den_now

    g0 = proj_steps(0)
    for _ in g0:
        pass
    for hh in range(H):
        g = proj_steps(hh + 1) if hh + 1 < H else None
        attention(hh, g)
        if g is not None:
            for _ in g:
                pass
    if pending_den[0] is not None:
        pending_den[0]()
        pending_den[0] = None
    flush(all_=True)

    sc.add("act", lambda h: h.activation(out=stat_sb[:, 0:NT], in_=ps[PB_STAT][:, 0:NT], func=AF.Ln, scale=1.0 / (CC * P), bias=EPS),
           reads=[("ps", PB_STAT)], writes=[("stat", 0)])
    sc.add("act", lambda h: h.activation(out=stat_sb[:, NT:2 * NT], in_=ps[PB_STAT][:, NT:2 * NT], func=AF.Ln, scale=1.0 / (H * P), bias=EPS),
           reads=[("ps", PB_STAT)], writes=[("stat", 1)])
    sc.add("act", lambda h: h.activation(out=stat_sb[:, :], in_=stat_sb[:, :], func=AF.Exp, scale=-0.5),
           reads=[("stat", 0), ("stat", 1)], writes=[("stat", 0), ("stat", 1)])

    HK = [k_hT(k, c) for k in range(KC) for c in range(NPC)]
    hk_deps = set()
    for k_ in HK:
        hk_deps |= set(sc.readers.get(k_, ()))
    first_use = set()

    def hk(key):
        if key in first_use:
            return ()
        first_use.add(key)
        return list(hk_deps)
    banks3 = rr([0, 1, 2, 3, 4, 5])
    x_t = x_d.rearrange("(t p) d -> t p d", p=P)
    o_t = out_d.rearrange("(t p) d -> t p d", p=P)
    outs = []
    iters = [(n, t) for n in range(ND) for t in range(NT)]
    PF = XR - 1

    def issue_x(i):
        n, t = iters[i]
        ld("sp", xres[:, i % XR, :], x_t[t, :, n * 512:(n + 1) * 512], [("xres", i % XR)], extra=hk(("xres", i % XR)))

    for i in range(min(PF, len(iters))):
        issue_x(i)
    for it, (n, t) in enumerate(iters):
        wb = n % 2
        if t == 0:
            ld("pool", wo[wb][:, :, :].rearrange("p e c -> p (e c)"), wout_d[n, :, :], [("wo", wb)], extra=hk(("wo", wb)))
        if it + PF < len(iters):
            issue_x(it + PF)
        rx = it % XR
        r3 = it % 3
        r2 = it % 2
        b1 = banks3()
        b2 = banks3()

        def f(h, t=t, wb=wb, b1=b1, b2=b2):
            ins = None
            for e in range(KC):
                bank = b1 if e < CC else b2
                ee = e if e < CC else e - CC
                last = (CC - 1) if e < CC else (KC - CC - 1)
                ins = h.matmul(ps[bank][:, :], lhsT=yT[:, e, t * P:(t + 1) * P], rhs=wo[wb][:, e, :], start=(ee == 0), stop=(ee == last))
            return ins
        sc.add("pe", f, reads=[("wo", wb)] + [k_yT(e, t // 4) for e in range(KC)], writes=[("ps", b1), ("ps", b2)])
        sc.add("dve", lambda h, t=t, b1=b1, rx=rx, r2=r2: h.scalar_tensor_tensor(
            out=t1[:, r2, :], in0=ps[b1][:, :], scalar=stat_sb[:, t:t + 1], in1=xres[:, rx, :], op0=ALU.mult, op1=ALU.add),
            reads=[("ps", b1), ("stat", 0), ("xres", rx)], writes=[("t1", r2)], extra=hk(("t1", r2)))
        sc.add("dve", lambda h, t=t, b2=b2, r3=r3, r2=r2: h.scalar_tensor_tensor(
            out=ot[:, r3, :], in0=ps[b2][:, :], scalar=stat_sb[:, NT + t:NT + t + 1], in1=t1[:, r2, :], op0=ALU.mult, op1=ALU.add),
            reads=[("ps", b2), ("stat", 1), ("t1", r2)], writes=[("ot", r3)], extra=hk(("ot", r3)))
        outs.append(sc.add("act", lambda h, t=t, n=n, r3=r3: h.dma_start(out=o_t[t, :, n * 512:(n + 1) * 512], in_=ot[:, r3, :]),
                           reads=[("ot", r3)], writes=[], dma=True))

    for name in dbg:
        src = {"hT": hT, "yT": yT, "qT": qT, "kT": kT, "Vt": Vt, "zs": zs, "negmT": negmT, "stat": stat_sb}[name]
        shp = list(src.shape)
        dt_ = BF16 if src.dtype == BF16 else F32
        d = nc.dram_tensor("dbg_" + name, shp, dt_, kind="ExternalOutput").ap()
        keys = {"hT": HK, "yT": [k_yT(e, c) for e in range(KC) for c in range(NPC)], "qT": [("qT", hp_, c) for hp_ in range(2) for c in range(NPC)],
                "kT": [("kT", hp_, c) for hp_ in range(2) for c in range(NPC)], "Vt": [("Vt", hp_, c) for hp_ in range(2) for c in range(NPC)], "zs": [("zs", hp_, c) for hp_ in range(2) for c in range(NPC)],
                "negmT": [("negmT", 0), ("negmT", 1)], "stat": [("stat", 0), ("stat", 1)]}[name]
        full = tuple(slice(None) for _ in shp)
        outs.append(sc.add("sp", lambda h, d=d, src=src, full=full: h.dma_start(out=d[full], in_=src[full]), reads=keys, dma=True))

    sc.add("sp", None, extra=outs, name="final")

    return _emit(nc, sc)


def _emit(nc, sc):
    import contextlib
    with contextlib.ExitStack() as es:
        csem = {e: es.enter_context(nc.semaphore("c_" + e)) for e in ("pe", "act", "dve", "pool")}
        dsems = {e: [es.enter_context(nc.semaphore("d_%s%d" % (e, i))) for i in range(NDMA)] for e in ("sp", "pool", "act")}
        dsems["dve"] = dsems["pe"] = []
        sc.finalize(csem, dsems)
        block = es.enter_context(nc.Block())

        @block.tensor
        def _(h):
            sc.emit("pe", h)

        @block.scalar
        def _(h):
            sc.emit("act", h)

        @block.vector
        def _(h):
            sc.emit("dve", h)

        @block.gpsimd
        def _(h):
            sc.emit("pool", h)

        @block.sync
        def _(h):
            sc.emit("sp", h)
    return nc


def make_kst():
    k = np.zeros((P, NKST), np.float32)
    k[:, K_ID:K_ID + P] = np.eye(P, dtype=np.float32)
    kk = np.arange(P)[:, None]
    qq = np.arange(P)[None, :]
    k[:, K_TRI:K_TRI + P] = np.where(kk <= qq, 0.0, -BIG)
    for j in range(NBLK):
        k[j, K_SEL + j * P:K_SEL + (j + 1) * P] = 1.0
    cand = np.zeros((8, 8), np.float32)
    fix = np.zeros((8, 8), np.float32)
    for tq in range(8):
        own = (8 + tq) // 2
        for j in range(8):
            cand[tq, j] = 0.0 if j < own else -1e30
            fix[tq, j] = 0.0 if j == own else -BIG
    k[:, K_CAND:K_CAND + 64] = cand.reshape(1, 64)
    k[:, K_FIX:K_FIX + 64] = fix.reshape(1, 64)
    return k


def host_layout(x, norm_gain, w_in, conv_w, q_norm_gain, k_norm_gain, conv_out_gain, attn_out_gain, w_out):
    B, S_, D = x.shape
    KC = D // P
    CC = KC // 2
    H = KC // 2
    NCH = 4 * KC
    ND = D // 512
    f = np.float32
    w_in_r = np.ascontiguousarray(np.asarray(w_in[0], f).reshape(KC, P, NCH, P).transpose(2, 1, 0, 3)).reshape(NCH, P, KC * P)
    w_out_r = np.ascontiguousarray(np.asarray(w_out[0], f).reshape(KC, P, ND, 512).transpose(2, 1, 0, 3)).reshape(ND, P, KC * 512)
    cst = np.concatenate([
        np.asarray(norm_gain[0], f).reshape(KC, P).T,
        np.asarray(conv_w[0], f).reshape(3, CC, P).transpose(2, 1, 0).reshape(P, 3 * CC),
        np.asarray(conv_out_gain[0], f).reshape(CC, P).T,
        np.asarray(attn_out_gain[0], f).reshape(H, P).T,
        np.asarray(q_norm_gain[0], f).reshape(P, 1),
        np.asarray(k_norm_gain[0], f).reshape(P, 1),
    ], axis=1)
    cst = np.ascontiguousarray(cst, dtype=f)
    kst = make_kst()
    maps = []
    for b in range(B):
        xb = np.ascontiguousarray(np.asarray(x[b], f))
        maps.append({"xT": np.ascontiguousarray(xb.T), "x": xb, "w_in": w_in_r, "w_out": w_out_r, "cst": cst, "kst": kst})
    return maps


_NC_CACHE = {}


def kernel(x, norm_gain, w_in, conv_w, q_norm_gain, k_norm_gain, conv_out_gain, attn_out_gain, w_out):
    x = np.asarray(x)
    B, S_, D = x.shape
    assert S_ == S
    maps = host_layout(x, np.asarray(norm_gain), np.asarray(w_in), np.asarray(conv_w), np.asarray(q_norm_gain),
                       np.asarray(k_norm_gain), np.asarray(conv_out_gain), np.asarray(attn_out_gain), np.asarray(w_out))
    nc = build(D)
    res = run_bass_kernel_spmd(nc, maps, core_ids=list(range(B)))
    return np.stack([np.asarray(r["out"], np.float32) for r in res.results], axis=0)
```
